# Optimizing a Trainium2 kernel written in Bass

```python
import jax, jax.numpy as jnp
from jax import lax
import numpy as np

D_MODEL = 2048
BATCH = 4
SEQ = 2048
DEPTH = 1

N_META = 16
GRID_W = 64
MIX_WIDTH = D_MODEL
N_HEADS = 16
HEAD_DIM = 64
ATTN_WIDTH = N_HEADS * HEAD_DIM
CONV_WIDTH = MIX_WIDTH - ATTN_WIDTH
CONV_K = 3
WIN_ROWS = 8
WIN_COLS = 16
N_GROUPS = 4
EXPERTS_PER_GROUP = 8
N_EXPERTS = N_GROUPS * EXPERTS_PER_GROUP
TOP_K = 2
EXPERT_FF = 1024
MOE_BLOCK = 128
EPS = 1e-6

PROJ_WIDTHS = (ATTN_WIDTH, ATTN_WIDTH, ATTN_WIDTH, CONV_WIDTH, CONV_WIDTH, CONV_WIDTH)
PROJ_SPLITS = tuple(int(s) for s in np.cumsum(PROJ_WIDTHS)[:-1])
PROJ_TOTAL = int(sum(PROJ_WIDTHS))

kernel_name = "hymba_na_shortconv_hmoe_encoder"


def rms_norm(x, w):
    xf = x.astype(jnp.float32)
    y = xf * lax.rsqrt(jnp.mean(xf * xf, axis=-1, keepdims=True) + EPS)
    return (y * w.astype(jnp.float32)).astype(x.dtype)


def neighbourhood_attention(q, k, v, rel_bias, meta_bias):
    bsz, length, nh, hd = q.shape
    rows = (length - N_META) // GRID_W
    wr = min(WIN_ROWS, rows)
    scale = hd ** -0.5
    qf = q.astype(jnp.float32) * scale
    kf = k.astype(jnp.float32)
    vf = v.astype(jnp.float32)
    qm, km, vm = qf[:, :N_META], kf[:, :N_META], vf[:, :N_META]
    qg = qf[:, N_META:].reshape(bsz, rows, GRID_W, nh, hd)
    kg = kf[:, N_META:].reshape(bsz, rows, GRID_W, nh, hd)
    vg = vf[:, N_META:].reshape(bsz, rows, GRID_W, nh, hd)

    r = jnp.arange(rows)
    c = jnp.arange(GRID_W)
    row_start = jnp.clip(r - wr // 2, 0, rows - wr)
    row_idx = row_start[:, None] + jnp.arange(wr)[None, :]
    col_start = jnp.clip(c - WIN_COLS // 2, 0, GRID_W - WIN_COLS)
    col_mask = (c[None, :] >= col_start[:, None]) & (c[None, :] < col_start[:, None] + WIN_COLS)

    k_rows = kg[:, row_idx]
    v_rows = vg[:, row_idx]

    dr = row_idx - r[:, None] + (WIN_ROWS - 1)
    dc = jnp.clip(c[None, :] - c[:, None], -(WIN_COLS - 1), WIN_COLS - 1) + (WIN_COLS - 1)
    bias = rel_bias.astype(jnp.float32)[:, dr[:, None, :, None], dc[None, :, None, :]]
    bias = jnp.where(col_mask[None, None, :, None, :], bias, -jnp.inf)

    s_grid = jnp.einsum('brqhd,brwkhd->bhrqwk', qg, k_rows) + bias[None]
    s_meta = jnp.einsum('brqhd,bmhd->bhrqm', qg, km) + meta_bias.astype(jnp.float32)[None, :, None, None, :]
    s = jnp.concatenate([s_grid.reshape(bsz, nh, rows, GRID_W, wr * GRID_W), s_meta], axis=-1)
    p = jax.nn.softmax(s, axis=-1)
    p_grid = p[..., :wr * GRID_W].reshape(bsz, nh, rows, GRID_W, wr, GRID_W)
    p_meta = p[..., wr * GRID_W:]
    o_grid = (jnp.einsum('bhrqwk,brwkhd->brqhd', p_grid, v_rows)
              + jnp.einsum('bhrqm,bmhd->brqhd', p_meta, vm))
    o_grid = o_grid.reshape(bsz, rows * GRID_W, nh, hd)

    p_mm = jax.nn.softmax(jnp.einsum('bmhd,bnhd->bhmn', qm, km), axis=-1)
    o_meta = jnp.einsum('bhmn,bnhd->bmhd', p_mm, vm)
    return jnp.concatenate([o_meta, o_grid], axis=1).astype(q.dtype)


def short_conv_mixer(gate_b, gate_c, h, conv_w):
    u = gate_c * h
    up = jnp.pad(u, ((0, 0), (1, 1), (0, 0)))
    y = up[:, :-2] * conv_w[0] + up[:, 1:-1] * conv_w[1] + up[:, 2:] * conv_w[2]
    return gate_b * y


def hierarchical_moe(x, w_rg, b_rg, w_re, b_re, w_gate, w_up, w_down):
    t, d = x.shape
    xf = x.astype(jnp.float32)
    g_prob = jax.nn.softmax(xf @ w_rg.astype(jnp.float32) + b_rg.astype(jnp.float32), axis=-1)
    g_idx = jnp.argmax(g_prob, axis=-1).astype(jnp.int32)
    g_w = jnp.max(g_prob, axis=-1)
    e_logits = (xf @ w_re.astype(jnp.float32) + b_re.astype(jnp.float32)).reshape(t, N_GROUPS, EXPERTS_PER_GROUP)
    e_logits = e_logits[jnp.arange(t), g_idx]
    top_p, top_j = lax.top_k(jax.nn.softmax(e_logits, axis=-1), TOP_K)
    weights = g_w[:, None] * top_p / jnp.sum(top_p, axis=-1, keepdims=True)
    experts = g_idx[:, None] * EXPERTS_PER_GROUP + top_j.astype(jnp.int32)

    n_assign = t * TOP_K
    e_flat = experts.reshape(-1)
    w_flat = weights.reshape(-1)
    tok_flat = jnp.repeat(jnp.arange(t, dtype=jnp.int32), TOP_K)
    order = jnp.argsort(e_flat)
    e_sorted = e_flat[order]
    counts = jnp.bincount(e_flat, length=N_EXPERTS)
    padded = (counts + MOE_BLOCK - 1) // MOE_BLOCK * MOE_BLOCK
    start_sorted = jnp.cumsum(counts) - counts
    seg_end = jnp.cumsum(padded)
    start_pad = seg_end - padded
    dest = start_pad[e_sorted] + (jnp.arange(n_assign) - start_sorted[e_sorted])
    n_blocks = (n_assign + N_EXPERTS * (MOE_BLOCK - 1) + MOE_BLOCK - 1) // MOE_BLOCK
    n_rows = n_blocks * MOE_BLOCK
    tok_buf = jnp.zeros((n_rows,), jnp.int32).at[dest].set(tok_flat[order])
    w_buf = jnp.zeros((n_rows,), jnp.float32).at[dest].set(w_flat[order])
    x_buf = x[tok_buf].reshape(n_blocks, MOE_BLOCK, d)
    block_expert = jnp.minimum(
        jnp.searchsorted(seg_end, jnp.arange(n_blocks) * MOE_BLOCK, side='right'), N_EXPERTS - 1)

    def expert_block(args):
        xb, e = args
        hb = jax.nn.silu(xb @ w_gate[e]) * (xb @ w_up[e])
        return hb @ w_down[e]

    y_buf = lax.map(expert_block, (x_buf, block_expert)).reshape(n_rows, d)
    y_buf = y_buf * w_buf[:, None].astype(y_buf.dtype)
    return jnp.zeros_like(x).at[tok_buf].add(y_buf.astype(x.dtype))


def setup_inputs(seed: int = 0) -> dict:
    key = jax.random.key(seed)
    ks = jax.random.split(key, 24)
    f32 = jnp.float32
    nrm = lambda k, shape, s: jax.random.normal(k, shape, f32) * s
    return {
        "x": nrm(ks[0], (BATCH, SEQ, D_MODEL), 1.0),
        "meta_tokens": nrm(ks[1], (N_META, D_MODEL), 1.0),
        "mix_norm_w": 1.0 + nrm(ks[2], (DEPTH, D_MODEL), 0.02),
        "w_in": nrm(ks[3], (DEPTH, D_MODEL, PROJ_TOTAL), D_MODEL ** -0.5),
        "q_norm_w": 1.0 + nrm(ks[4], (DEPTH, HEAD_DIM), 0.02),
        "k_norm_w": 1.0 + nrm(ks[5], (DEPTH, HEAD_DIM), 0.02),
        "rel_bias": nrm(ks[6], (DEPTH, N_HEADS, 2 * WIN_ROWS - 1, 2 * WIN_COLS - 1), 0.1),
        "meta_bias": nrm(ks[7], (DEPTH, N_HEADS, N_META), 0.1),
        "conv_w": nrm(ks[8], (DEPTH, CONV_K, CONV_WIDTH), CONV_K ** -0.5),
        "attn_out_norm_w": 1.0 + nrm(ks[9], (DEPTH, ATTN_WIDTH), 0.02),
        "conv_out_norm_w": 1.0 + nrm(ks[10], (DEPTH, CONV_WIDTH), 0.02),
        "w_out": nrm(ks[11], (DEPTH, MIX_WIDTH, D_MODEL), MIX_WIDTH ** -0.5),
        "ffn_norm_w": 1.0 + nrm(ks[12], (DEPTH, D_MODEL), 0.02),
        "w_router_group": nrm(ks[13], (DEPTH, D_MODEL, N_GROUPS), D_MODEL ** -0.5),
        "b_router_group": nrm(ks[14], (DEPTH, N_GROUPS), 0.01),
        "w_router_expert": nrm(ks[15], (DEPTH, D_MODEL, N_EXPERTS), D_MODEL ** -0.5),
        "b_router_expert": nrm(ks[16], (DEPTH, N_EXPERTS), 0.01),
        "w_gate": nrm(ks[17], (DEPTH, N_EXPERTS, D_MODEL, EXPERT_FF), D_MODEL ** -0.5),
        "w_up": nrm(ks[18], (DEPTH, N_EXPERTS, D_MODEL, EXPERT_FF), D_MODEL ** -0.5),
        "w_down": nrm(ks[19], (DEPTH, N_EXPERTS, EXPERT_FF, D_MODEL), EXPERT_FF ** -0.5),
    }


def reference(x, meta_tokens, mix_norm_w, w_in, q_norm_w, k_norm_w, rel_bias, meta_bias, conv_w,
              attn_out_norm_w, conv_out_norm_w, w_out, ffn_norm_w, w_router_group, b_router_group,
              w_router_expert, b_router_expert, w_gate, w_up, w_down):
    bsz, seq, d = x.shape
    meta = jnp.broadcast_to(meta_tokens[None].astype(x.dtype), (bsz, N_META, d))
    h = jnp.concatenate([meta, x], axis=1)
    length = h.shape[1]
    for l in range(DEPTH):
        xn = rms_norm(h, mix_norm_w[l])
        proj = xn @ w_in[l]
        q, k, v, gate_b, gate_c, hc = jnp.split(proj, PROJ_SPLITS, axis=-1)
        q = rms_norm(q.reshape(bsz, length, N_HEADS, HEAD_DIM), q_norm_w[l])
        k = rms_norm(k.reshape(bsz, length, N_HEADS, HEAD_DIM), k_norm_w[l])
        v = v.reshape(bsz, length, N_HEADS, HEAD_DIM)
        a = neighbourhood_attention(q, k, v, rel_bias[l], meta_bias[l]).reshape(bsz, length, ATTN_WIDTH)
        c = short_conv_mixer(gate_b, gate_c, hc, conv_w[l])
        mixed = jnp.concatenate([rms_norm(a, attn_out_norm_w[l]), rms_norm(c, conv_out_norm_w[l])], axis=-1)
        h = h + mixed @ w_out[l]
        hn = rms_norm(h, ffn_norm_w[l]).reshape(bsz * length, d)
        h = h + hierarchical_moe(hn, w_router_group[l], b_router_group[l], w_router_expert[l],
                                 b_router_expert[l], w_gate[l], w_up[l], w_down[l]).reshape(bsz, length, d)
    return h[:, N_META:]
```

```python
from contextlib import ExitStack

import numpy as np
import concourse.bass as bass
import concourse.mybir as mybir
from concourse.bass_utils import run_bass_kernel_spmd

F32 = mybir.dt.float32
BF16 = mybir.dt.bfloat16
AF = mybir.ActivationFunctionType
ALU = mybir.AluOpType
AX = mybir.AxisListType

EPS = 1e-6
NEG = -30000.0
NE = 32
SLOTS = [[0, 1, 2, 3, 'A', 'B'], [0, 1, 2, 3, 'B'], [0, 1, 2, 3, 4], [1, 2, 3, 4, 5],
         [2, 3, 4, 5, 6], [3, 4, 5, 6, 7], [4, 5, 6, 7, 'A'], [4, 5, 6, 7, 'A', 'B']]
SLOT_BASE = np.cumsum([0] + [len(s) for s in SLOTS]).tolist()
NSLOT = SLOT_BASE[-1]
ENG = ['pe', 'act', 'dve', 'pool', 'sp']
C_WQ, C_WK, C_CW, C_WC, C_IOP, C_ONE = 0, 1, 2, 26, 34, 35
NCP = 36
HORDER = [8 * (u // 2) + 2 * i + (u % 2) for u in range(4) for i in range(4)]


class Prog:
    def __init__(self):
        self.ops = {e: [] for e in ENG}
        self.cnt = {e: 0 for e in ENG}
        self.seen = {e: {} for e in ENG}
        self.lastw = {}
        self.readers = {}
        self.dsem = {}
        self.pending = {e: [] for e in ENG}

    def _deps(self, eng, reads, writes):
        deps = list(self.pending[eng])
        self.pending[eng] = []
        for k in reads:
            deps += self.lastw.get(k, [])
        for k in writes:
            deps += self.lastw.get(k, [])
            deps += self.readers.get(k, [])
        return deps

    def _waits(self, eng, deps):
        waits = []
        best = {}
        for (s, v) in deps:
            if s == eng and eng == 'pe':
                continue
            if self.seen[eng].get(s, 0) >= v:
                continue
            best[s] = max(best.get(s, 0), v)
        for s, v in best.items():
            self.seen[eng][s] = v
            waits.append((s, v))
        return waits

    def _record(self, evs, reads, writes):
        for k in writes:
            self.lastw[k] = list(evs)
            self.readers[k] = []
        for k in reads:
            self.readers.setdefault(k, []).extend(evs)

    def op(self, eng, fn, reads=(), writes=(), mark=True):
        assert mark or eng == 'pe'
        waits = self._waits(eng, self._deps(eng, reads, writes))
        if mark:
            self.cnt[eng] += 1
            ev = (eng, self.cnt[eng])
        else:
            ev = (eng, self.cnt[eng] + 1)
        self.ops[eng].append((fn, waits, eng if mark else None, 1))
        self._record([ev], reads, writes)
        return ev

    def dma(self, q, fn, sem, reads=(), writes=()):
        return self.dma_group(q, [fn], [sem], reads, writes)

    def dma_group(self, q, fns, sems, reads=(), writes=()):
        deps = self._deps(q, reads, writes)
        for sem in sems:
            cur = self.dsem.get(sem, 0)
            if cur > 0:
                deps.append((sem, cur))
        waits = self._waits(q, deps)
        evs = []
        for i, (fn, sem) in enumerate(zip(fns, sems)):
            self.dsem[sem] = self.dsem.get(sem, 0) + 16
            evs.append((sem, self.dsem[sem]))
            self.ops[q].append((fn, waits if i == 0 else [], sem, 16))
        self._record(evs, reads, writes)
        return evs

    def barrier(self):
        evs = [(e, self.cnt[e]) for e in ENG if self.cnt[e] > 0]
        evs += [(s, v) for s, v in self.dsem.items()]
        for e in ENG:
            self.pending[e] = list(evs)
        self.lastw = {}
        self.readers = {}

    def sem_names(self):
        return list(ENG) + list(self.dsem.keys())


def build(moe=True, stage=9):
    nc = bass.Bass("TRN2", target_bir_lowering=False)

    def din(name, shape):
        return nc.dram_tensor(name, shape, F32, kind="ExternalInput").ap()

    xo = din("xo", [1024, 2048])
    xg = din("xg", [288, 2048])
    w_in = din("w_in", [2048, 6144])
    w_out = din("w_out", [2048, 2048])
    tab = din("tab", [NSLOT, 128, 2048])
    tabm = din("tabm", [16, 2048])
    cpack = din("cpack", [128, NCP])
    wbc_mix = din("wbc_mix", [128, 2048])
    wbc_attn = din("wbc_attn", [128, 1024])
    wbc_ffn = din("wbc_ffn", [128, 2048])
    brt = din("brt", [128, 288])
    wr = din("wr", [128, 16 * 36])
    cmat = din("cmat", [128, 128 * 3])
    ccm = din("ccm", [128, 1920])
    rsel = din("rsel", [32, NE * 128])
    if moe:
        w_gate = din("w_gate", [NE, 2048, 1024])
        w_up = din("w_up", [NE, 2048, 1024])
        w_down = din("w_down", [NE, 1024, 2048])
    out = nc.dram_tensor("out", [1024, 2048], F32, kind="ExternalOutput").ap()

    P = Prog()
    TOT = 53000
    with ExitStack() as es:
        arena = es.enter_context(nc.sbuf_tensor("arena", [128, TOT], F32))
        ps = es.enter_context(nc.psum_tensor("ps", [128, 8, 512], F32))

        def A(off, n):
            assert off + n <= TOT, (off, n)
            return arena[:, off:off + n]

        def AB(off, n):
            return A(off, n).bitcast(BF16)

        def psb(b):
            return ps[:, b, :].bitcast(BF16)

        o = 0
        cp = A(o, NCP); o += NCP
        identf = A(o, 128); o += 128
        bones = A(o, 128); o += 128
        iotar = A(o, 128); o += 128
        identb = AB(o, 64); o += 64
        st = A(o, 160); o += 160
        brt_s = A(o, 288); o += 288
        wq8 = A(o, 1); o += 1
        CONST_END = 2560
        assert o <= CONST_END
        ss, rstd = st[:, 0:16], st[:, 16:32]
        ssa, ra, rc = st[:, 32:40], st[:, 40:48], st[:, 48:56]
        ssf, rf = st[:, 56:64], st[:, 64:72]
        rec4 = st[:, 72:88]

        P.dma('sp', lambda e: e.dma_start(out=cp, in_=cpack[:, :]), 'k1', writes=['const'])
        P.dma('sp', lambda e: e.dma_start(out=identf, in_=cmat[:, 0:128]), 'k2', writes=['const'])
        P.dma('sp', lambda e: e.dma_start(out=bones, in_=cmat[:, 128:256]), 'k3', writes=['const'])
        P.dma('sp', lambda e: e.dma_start(out=iotar, in_=cmat[:, 256:384]), 'k4', writes=['const'])
        P.dma('sp', lambda e: e.dma_start(out=brt_s, in_=brt[:, :]), 'k5', writes=['const'])
        P.dma('pool', lambda e: e.dma_start(out=identb, in_=cmat[:, 0:128]), 'k6', writes=['const'])

        R1 = CONST_END
        o = R1
        qT_o = o; o += 4096
        kT_o = o; o += 5248
        V_o = o; o += 5808
        U_o = o; o += 8208
        R2 = o
        cT_o = o; o += 4096
        R3 = o
        xTo_o = o; o += 8192
        xTg_o = o; o += 2304
        wb_o = [o, o + 4096]; o += 8192
        assert o <= TOT, o
        qT = AB(qT_o, 4096).rearrange("p (c t) -> p c t", c=8)
        kT = AB(kT_o, 5248).rearrange("p (c t) -> p c t", c=8)
        Vv = AB(V_o, 5808).rearrange("p (c h d) -> p c h d", c=11, h=16)
        U = A(U_o, 8208).rearrange("p (c t) -> p c t", c=8)
        cT = AB(cT_o, 4096).rearrange("p (c t) -> p c t", c=8)
        xTo = AB(xTo_o, 8192).rearrange("p (k t) -> p k t", k=16)
        xTg = AB(xTg_o, 2304).rearrange("p (k t) -> p k t", k=16)
        wblk = [AB(wb_o[i], 4096).rearrange("p (k c) -> p k c", k=16) for i in range(2)]
        xst = [A(U_o + i * 2048, 2048) for i in range(2)]
        xsb = [AB(U_o + 4096 + i * 1024, 1024) for i in range(2)]
        wmix = A(U_o + 6144, 2048)
        junk = AB(cT_o, 1024)

        P.dma('sp', lambda e: e.dma_start(out=wmix, in_=wbc_mix[:, :]), 'k7', writes=['wmix'])
        P.op('dve', lambda e: e.tensor_scalar(wq8, cp[:, C_WQ:C_WQ + 1], 0.125, None, ALU.mult),
             reads=['const'], writes=['wq8'])

        blocks_enabled = stage >= 2
        blocks = [('q', 0), ('q', 1), ('k', 0), ('k', 1), ('v', 0), ('v', 1),
                  ('C', 0), ('hc', 0), ('C', 1), ('hc', 1), ('B', 0), ('B', 1)]
        colbase = {'q': 0, 'k': 1024, 'v': 2048, 'B': 3072, 'C': 4096, 'hc': 5120}
        w_in_v = w_in.rearrange("(k p) c -> p k c", p=128)

        def load_wblk(bi):
            typ, half = blocks[bi]
            c0 = colbase[typ] + half * 512
            s = bi % 2
            P.dma_group('pool', [lambda e, s=s, c0=c0, q4=q4: e.dma_start(
                out=wblk[s][:, q4 * 4:(q4 + 1) * 4, :], in_=w_in_v[:, q4 * 4:(q4 + 1) * 4, c0:c0 + 512]) for q4 in range(4)],
                ['wb%d_%d' % (s, q4) for q4 in range(4)], writes=[('wblk', s)])

        if blocks_enabled:
            load_wblk(0)
            load_wblk(1)
        tiles = [(xo, j * 128, 128, xTo, j * 128) for j in range(8)]
        tiles += [(xg, 0, 128, xTg, 0), (xg, 128, 128, xTg, 128), (xg, 256, 32, xTg, 256)]
        for t, (src, r0, R, dst, c0) in enumerate(tiles):
            s = t % 2
            P.dma('sp', lambda e, s=s, src=src, r0=r0, R=R: e.dma_start(out=xst[s][:R, :], in_=src[r0:r0 + R, :]),
                  'xld%d' % s, writes=[('xst', s)])
            P.op('act', lambda e, s=s, R=R, t=t: e.activation(junk[:R, :], xst[s][:R, :], AF.Square,
                                                              accum_out=ss[:R, t:t + 1]),
                 reads=[('xst', s)], writes=['junk', ('ss', t)])
            P.op('act', lambda e, R=R, t=t: e.activation(rstd[:R, t:t + 1], ss[:R, t:t + 1], AF.Sqrt, bias=EPS, scale=1.0 / 2048),
                 reads=[('ss', t)], writes=[('rstd', t)])
            P.op('dve', lambda e, R=R, t=t: e.reciprocal(rstd[:R, t:t + 1], rstd[:R, t:t + 1]),
                 reads=[('rstd', t)], writes=[('rstd', t)])
            P.op('dve', lambda e, s=s, R=R, t=t: e.scalar_tensor_tensor(xsb[s][:R, :], xst[s][:R, :], rstd[:R, t:t + 1],
                                                                        wmix[:R, :], ALU.mult, ALU.mult),
                 reads=[('xst', s), ('rstd', t), 'wmix'], writes=[('xsb', s)])
            for hb in range(2):
                bank = (t % 2) * 2 + hb
                for kk in range(8):
                    k = hb * 8 + kk
                    P.op('pe', lambda e, s=s, R=R, k=k, kk=kk, bank=bank: e.transpose(
                        psb(bank)[:, kk * 128:kk * 128 + R], xsb[s][:R, k * 128:(k + 1) * 128], identb[:R, :R]),
                        reads=[('xsb', s), 'const'], writes=[('ps', bank)], mark=(kk == 7))
                src_ps = psb(bank).rearrange("p (k t) -> p k t", k=8)[:, :, 0:R]
                dst_ap = dst[:, hb * 8:hb * 8 + 8, c0:c0 + R]
                if hb == 0:
                    P.op('act', lambda e, a=dst_ap, b=src_ps: e.copy(a, b), reads=[('ps', bank)], writes=['xT'])
                else:
                    P.op('dve', lambda e, a=dst_ap, b=src_ps: e.tensor_copy(a, b), reads=[('ps', bank)], writes=['xT'])
        P.barrier()

        P.op('dve', lambda e: e.memset(Vv[:, :, :, 64:66], 1.0), writes=['V'])
        tl = wb_o[1] + 4096
        sqb = [A(tl, 512), A(tl + 512, 512)]
        rsb = [A(tl + 1024, 512), A(tl + 1536, 512)]
        tb = [A(tl + 2048, 512), A(tl + 2560, 512)]
        assert tl + 3072 <= TOT, tl
        bankrot = [0]
        dpe = []
        srot = [0]
        urot = [0]

        def nextbank():
            b = bankrot[0] % 5
            bankrot[0] += 1
            return b

        for bi, (typ, half) in enumerate(blocks if blocks_enabled else []):
            s = bi % 2
            wb = wblk[s]
            if typ == 'v':
                vt = [(xTo, j * 128, 128, j) for j in range(8)] + [(xTg, 0, 128, 8), (xTg, 128, 128, 9), (xTg, 256, 32, 10)]
                for (xt, c0, R, ch) in vt:
                    b = nextbank()
                    for k in range(16):
                        P.op('pe', lambda e, b=b, xt=xt, c0=c0, R=R, k=k, wb=wb: e.matmul(
                            ps[:R, b, :], xt[:, k, c0:c0 + R], wb[:, k, :], start=(k == 0), stop=(k == 15)),
                            reads=[('wblk', s), 'xT'], writes=[('ps', b)], mark=(k == 15))
                    while dpe:
                        dpe.pop(0)()
                    P.op('act', lambda e, b=b, R=R, ch=ch, half=half: e.copy(
                        Vv[:R, ch, half * 8:half * 8 + 8, 0:64], ps[:R, b, :].rearrange("p (h d) -> p h d", h=8)),
                        reads=[('ps', b)], writes=['V'])
            else:
                for ct in range(4):
                    gct = half * 4 + ct
                    groups = [(xTo, 0, 512, 0), (xTo, 512, 512, 512)]
                    if typ == 'k':
                        groups.append((xTg, 0, 288, 1024))
                    if typ in ('C', 'hc'):
                        groups.append((xTg, 272, 2, -1))
                    for (xt, c0, N, d0) in groups:
                        b = nextbank()
                        for k in range(16):
                            P.op('pe', lambda e, b=b, xt=xt, c0=c0, N=N, k=k, wb=wb, ct=ct: e.matmul(
                                ps[:, b, 0:N], wb[:, k, ct * 128:(ct + 1) * 128], xt[:, k, c0:c0 + N],
                                start=(k == 0), stop=(k == 15)),
                                reads=[('wblk', s), 'xT'], writes=[('ps', b)], mark=(k == 15))
                        while dpe:
                            dpe.pop(0)()
                        if typ in ('q', 'k'):
                            u = urot[0] % 2
                            urot[0] += 1
                            sb = 5 + (srot[0] % 2)
                            srot[0] += 1
                            P.op('act', lambda e, b=b, N=N, u=u: e.activation(sqb[u][:, 0:N], ps[:, b, 0:N], AF.Square),
                                 reads=[('ps', b)], writes=[('sq', u)])
                            dstT = qT if typ == 'q' else kT
                            wcol = wq8 if typ == 'q' else cp[:, C_WK:C_WK + 1]

                            def post(sb=sb, N=N, u=u, b=b, dstT=dstT, wcol=wcol, gct=gct, d0=d0):
                                P.op('pe', lambda e: e.matmul(ps[:, sb, 0:N], bones, sqb[u][:, 0:N], start=True, stop=True),
                                     reads=[('sq', u), 'const'], writes=[('ps', sb)])
                                P.op('act', lambda e: e.activation(rsb[u][:, 0:N], ps[:, sb, 0:N], AF.Sqrt,
                                                                   bias=EPS, scale=1.0 / 64),
                                     reads=[('ps', sb)], writes=[('rs', u)])
                                P.op('dve', lambda e: e.reciprocal(rsb[u][:, 0:N], rsb[u][:, 0:N]),
                                     reads=[('rs', u)], writes=[('rs', u)])
                                P.op('dve', lambda e: e.scalar_tensor_tensor(
                                    dstT[:, gct, d0:d0 + N], ps[:, b, 0:N], wcol, rsb[u][:, 0:N], ALU.mult, ALU.mult),
                                    reads=[('ps', b), ('rs', u), 'wq8', 'const'], writes=['qkT'])
                            dpe.append(post)
                        elif typ == 'C':
                            if d0 >= 0:
                                P.op('act', lambda e, b=b, gct=gct, d0=d0: e.copy(U[:, gct, 1 + d0:1 + d0 + 512], ps[:, b, :]),
                                     reads=[('ps', b)], writes=[('U', gct)])
                            else:
                                P.op('act', lambda e, b=b, gct=gct: e.copy(U[:, gct, 0:1], ps[:, b, 0:1]),
                                     reads=[('ps', b)], writes=[('U', gct)])
                                P.op('act', lambda e, b=b, gct=gct: e.copy(U[:, gct, 1025:1026], ps[:, b, 1:2]),
                                     reads=[('ps', b)], writes=[('U', gct)])
                        elif typ == 'hc':
                            if d0 >= 0:
                                P.op('dve', lambda e, b=b, gct=gct, d0=d0: e.tensor_tensor(
                                    U[:, gct, 1 + d0:1 + d0 + 512], U[:, gct, 1 + d0:1 + d0 + 512], ps[:, b, :], ALU.mult),
                                    reads=[('ps', b), ('U', gct)], writes=[('U', gct)])
                            else:
                                P.op('dve', lambda e, b=b, gct=gct: e.tensor_tensor(
                                    U[:, gct, 0:1], U[:, gct, 0:1], ps[:, b, 0:1], ALU.mult),
                                    reads=[('ps', b), ('U', gct)], writes=[('U', gct)])
                                P.op('dve', lambda e, b=b, gct=gct: e.tensor_tensor(
                                    U[:, gct, 1025:1026], U[:, gct, 1025:1026], ps[:, b, 1:2], ALU.mult),
                                    reads=[('ps', b), ('U', gct)], writes=[('U', gct)])
                        else:
                            u = urot[0] % 2
                            urot[0] += 1
                            n0 = d0
                            cw = lambda tap, gct=gct: cp[:, C_CW + gct * 3 + tap:C_CW + gct * 3 + tap + 1]
                            P.op('dve', lambda e, u=u, gct=gct, n0=n0, cw=cw: e.tensor_scalar(
                                tb[u], U[:, gct, n0:n0 + 512], cw(0), None, ALU.mult),
                                reads=[('U', gct), 'const'], writes=[('tb', u)])
                            P.op('dve', lambda e, u=u, gct=gct, n0=n0, cw=cw: e.scalar_tensor_tensor(
                                tb[u], U[:, gct, n0 + 1:n0 + 513], cw(1), tb[u], ALU.mult, ALU.add),
                                reads=[('U', gct), ('tb', u)], writes=[('tb', u)])
                            P.op('dve', lambda e, u=u, gct=gct, n0=n0, cw=cw: e.scalar_tensor_tensor(
                                tb[u], U[:, gct, n0 + 2:n0 + 514], cw(2), tb[u], ALU.mult, ALU.add),
                                reads=[('U', gct), ('tb', u)], writes=[('tb', u)])
                            P.op('dve', lambda e, u=u, gct=gct, b=b: e.scalar_tensor_tensor(
                                tb[u], ps[:, b, :], cp[:, C_WC + gct:C_WC + gct + 1], tb[u], ALU.mult, ALU.mult),
                                reads=[('ps', b), ('tb', u)], writes=[('tb', u)])
                            P.op('act', lambda e, u=u, gct=gct, n0=n0: e.copy(cT[:, gct, n0:n0 + 512], tb[u]),
                                 reads=[('tb', u)], writes=['cT'])
                            P.op('act', lambda e, u=u: e.activation(sqb[u], tb[u], AF.Square),
                                 reads=[('tb', u)], writes=[('sq', u)])
                            def postb(u=u, n0=n0, gct=gct):
                              for tt in range(4):
                                j = n0 // 128 + tt
                                P.op('pe', lambda e, u=u, tt=tt, j=j, gct=gct: e.matmul(
                                    ps[:, 7, j:j + 1], sqb[u][:, tt * 128:(tt + 1) * 128], cp[:, C_ONE:C_ONE + 1],
                                    start=(gct == 0 and j == 0), stop=(gct == 7), skip_group_check=True),
                                    reads=[('sq', u), 'const'], writes=[('ps', 7)], mark=(tt == 3))
                            dpe.append(postb)
            if bi + 2 < len(blocks):
                load_wblk(bi + 2)
        while dpe:
            dpe.pop(0)()
        P.op('act', lambda e: e.activation(rc, ps[:, 7, 0:8], AF.Sqrt, bias=EPS, scale=1.0 / 1024),
             reads=[('ps', 7)], writes=['rc'])
        P.op('dve', lambda e: e.reciprocal(rc, rc), reads=['rc'], writes=['rc'])
        P.barrier()

        o = R3
        aT_o = o; o += 4096
        tab_o = [o, o + 2048]; o += 4096
        tabm_o = o; o += 2048
        sc_o = [o + 512 * i for i in range(4)]; o += 2048
        pT_o = [o + 256 * i for i in range(4)]; o += 1024
        at_o = o; o += 1024
        abf_o = o; o += 512
        wattn_o = o; o += 1024
        junk2_o = o; o += 512
        assert o <= TOT
        aT = AB(aT_o, 4096).rearrange("p (c t) -> p c t", c=8)
        tabs = [A(tab_o[i], 2048) for i in range(2)]
        tabms = A(tabm_o, 2048)
        scs = [A(sc_o[i], 512) for i in range(4)]
        pTs = [AB(pT_o[i], 256) for i in range(4)]
        a_t = A(at_o, 1024)
        a_bf = AB(abf_o, 512)
        wattn = A(wattn_o, 1024)
        junk2 = AB(junk2_o, 512)
        P.dma('sp', lambda e: e.dma_start(out=tabms[0:16, :], in_=tabm[:, :]), 'k8', writes=['tabm'])
        P.dma('sp', lambda e: e.dma_start(out=wattn, in_=wbc_attn[:, :]), 'k9', writes=['wattn'])
        slot_list = []
        for j in range(8):
            for si, kc in enumerate(SLOTS[j]):
                slot_list.append((j, si, kc, SLOT_BASE[j] + si))

        def load_tab(n):
            gi = slot_list[n][3]
            s = n % 2
            P.dma('sp', lambda e, s=s, gi=gi: e.dma_start(out=tabs[s], in_=tab[gi, :, :]), 'tab%d' % s,
                  writes=[('tab', s)])

        wo_o = [R1 + 16384, aT_o + 4096]
        assert wo_o[0] + 4096 + 1024 <= R2
        wob = [AB(wo_o[i], 4096).rearrange("p (k c) -> p k c", k=16) for i in range(2)]
        junkO = AB(wo_o[0] + 4096, 1024)
        w_out_v = w_out.rearrange("(k p) c -> p k c", p=128)

        def load_wo(cb):
            s = cb % 2
            P.dma_group('pool', [lambda e, s=s, cb=cb, q4=q4: e.dma_start(
                out=wob[s][:, q4 * 4:(q4 + 1) * 4, :], in_=w_out_v[:, q4 * 4:(q4 + 1) * 4, cb * 512:(cb + 1) * 512]) for q4 in range(4)],
                ['wo%d_%d' % (s, q4) for q4 in range(4)], writes=[('wo', s)])

        if stage >= 4:
            load_wo(0)
        if stage >= 3:
            load_tab(0)
            load_tab(1)
        n = 0
        scr = [0]
        pend = []
        for j in range(8 if stage >= 3 else 0):
            entries = [(kc, False) for kc in SLOTS[j]] + [('M', True)]
            for si, (kc, is_meta) in enumerate(entries):
                first = (si == 0)
                last = is_meta
                if is_meta:
                    kcol, KR, vch, tb_ap, tkey = 1280, 16, 10, tabms, 'tabm'
                else:
                    if kc == 'A':
                        kcol, vch = 1024, 8
                    elif kc == 'B':
                        kcol, vch = 1152, 9
                    else:
                        kcol, vch = kc * 128, kc
                    KR = 128
                    ts_ = n % 2
                    tb_ap, tkey = tabs[ts_], ('tab', ts_)
                for hg in range(4):
                    sbk = scr[0] % 4
                    u = scr[0] % 4
                    scr[0] += 1
                    p0 = 64 * (hg % 2)
                    for h4 in range(4):
                        ct_ = 4 * (hg // 2) + h4
                        P.op('pe', lambda e, sbk=sbk, h4=h4, ct_=ct_, p0=p0, kcol=kcol, KR=KR, j=j: e.matmul(
                            ps[:KR, sbk, h4 * 128:(h4 + 1) * 128], kT[p0:p0 + 64, ct_, kcol:kcol + KR],
                            qT[p0:p0 + 64, ct_, j * 128:(j + 1) * 128], start=True, stop=True),
                            reads=['qkT'], writes=[('ps', sbk)], mark=(h4 == 3))
                    P.op('dve', lambda e, sbk=sbk, u=u, KR=KR, tb_ap=tb_ap, hg=hg: e.tensor_tensor(
                        scs[u][:KR, :], ps[:KR, sbk, :], tb_ap[:KR, hg * 512:(hg + 1) * 512], ALU.add),
                        reads=[('ps', sbk), tkey], writes=[('sc', u)])
                    P.op('act', lambda e, u=u, KR=KR: e.activation(pTs[u][:KR, :], scs[u][:KR, :], AF.Exp),
                         reads=[('sc', u)], writes=[('pT', u)])

                    def emit_pv(u=u, KR=KR, vch=vch, hg=hg, first=first, last=last):
                        for h4 in range(4):
                            h = 8 * (hg // 2) + 2 * h4 + (hg % 2)
                            P.op('pe', lambda e, u=u, h4=h4, h=h, KR=KR, vch=vch, hg=hg, first=first, last=last: e.matmul(
                                ps[:, 4 + hg, h4 * 66:(h4 + 1) * 66], pTs[u][:KR, h4 * 128:(h4 + 1) * 128],
                                Vv[:KR, vch, h, :], start=(first and h4 == 0), stop=last, skip_group_check=True),
                                reads=[('pT', u), 'V'], writes=[('ps', 4 + hg)], mark=(h4 == 3))
                    pend.append(emit_pv)
                    while sum(1 for f in pend if f.__name__ == 'emit_pv') > 2:
                        pend.pop(0)()
                if not is_meta:
                    n += 1
                    if n + 1 < len(slot_list):
                        load_tab(n + 1)
            def norm(j=j):
                for hg in range(4):
                    ov = ps[:, 4 + hg, 0:264].rearrange("p (h d) -> p h d", h=4)
                    P.op('dve', lambda e, ov=ov, hg=hg: e.reciprocal(rec4[:, hg * 4:hg * 4 + 4].rearrange("p (h o) -> p h o", o=1), ov[:, :, 64:65]),
                         reads=[('ps', 4 + hg)], writes=[('rec', hg)])
                    abase = (8 * (hg // 2) + (hg % 2)) * 64
                    P.op('dve', lambda e, ov=ov, hg=hg, abase=abase: e.tensor_tensor(
                        a_t.rearrange("p (g x) -> p g x", x=128)[:, 4 * (hg // 2):4 * (hg // 2) + 4, (hg % 2) * 64:(hg % 2) * 64 + 64], ov[:, :, 0:64],
                        rec4[:, hg * 4:hg * 4 + 4].rearrange("p (h o) -> p h o", o=1).to_broadcast([128, 4, 64]), ALU.mult),
                        reads=[('ps', 4 + hg), ('rec', hg)], writes=['a_t'])
                P.op('act', lambda e, j=j: e.activation(junk2, a_t, AF.Square, accum_out=ssa[:, j:j + 1]),
                     reads=['a_t'], writes=['junk2', ('ssa', j)])
                P.op('dve', lambda e: e.tensor_tensor(a_bf, a_t, wattn, ALU.mult), reads=['a_t', 'wattn'], writes=['a_bf'])
                tbk = scr[0] % 4
                scr[0] += 1
                for c8 in range(8):
                    P.op('pe', lambda e, tbk=tbk, c8=c8: e.transpose(psb(tbk)[:, c8 * 128:(c8 + 1) * 128],
                                                                     a_bf[:, c8 * 128:(c8 + 1) * 128], identb),
                         reads=['a_bf', 'const'], writes=[('ps', tbk)], mark=(c8 == 7))
                P.op('act', lambda e, tbk=tbk, j=j: e.copy(aT[:, :, j * 128:(j + 1) * 128],
                                                            psb(tbk).rearrange("p (c t) -> p c t", c=8)),
                     reads=[('ps', tbk)], writes=['aT'])
            pend.append(norm)
        while pend:
            pend.pop(0)()
        P.op('act', lambda e: e.activation(ra, ssa, AF.Sqrt, bias=EPS, scale=1.0 / 1024),
             reads=[('ssa', j) for j in range(8)], writes=['ra'])
        P.op('dve', lambda e: e.reciprocal(ra, ra), reads=['ra'], writes=['ra'])
        P.barrier()

        h1 = A(R1, 16384).rearrange("p (j d) -> p j d", j=8)
        o = aT_o + 4096
        o += 8192
        xc_o = [o, o + 512]; o += 1024
        tmp_o = o; o += 512
        assert o <= TOT
        xcs = [A(xc_o[i], 512) for i in range(2)]
        tmpc = A(tmp_o, 512)

        RING_FIX = TOT - 8 * 1024
        assert o <= RING_FIX, o
        if stage >= 4:
            load_wo(1)
        moe_state = None
        if moe:
            moe_state = moe_ring(P, AB, RING_FIX, R1 + 16384, w_gate, w_up, w_down)
            for ci in range(8):
                moe_state['load'](ci)
        it = 0
        for cb in range(4 if stage >= 4 else 0):
            s = cb % 2
            for j in range(8):
                xs_ = it % 2
                P.dma('sp', lambda e, xs_=xs_, j=j, cb=cb: e.dma_start(
                    out=xcs[xs_], in_=xo[j * 128:(j + 1) * 128, cb * 512:(cb + 1) * 512]), 'xc%d' % xs_,
                    writes=[('xc', xs_)])
                ba, bc = (it % 4) * 2, (it % 4) * 2 + 1
                for k in range(8):
                    P.op('pe', lambda e, ba=ba, k=k, j=j, s=s: e.matmul(
                        ps[:, ba, :], aT[:, k, j * 128:(j + 1) * 128], wob[s][:, k, :], start=(k == 0), stop=(k == 7)),
                        reads=[('wo', s), 'aT'], writes=[('ps', ba)], mark=(k == 7))
                for k in range(8):
                    P.op('pe', lambda e, bc=bc, k=k, j=j, s=s: e.matmul(
                        ps[:, bc, :], cT[:, k, j * 128:(j + 1) * 128], wob[s][:, 8 + k, :], start=(k == 0), stop=(k == 7)),
                        reads=[('wo', s), 'cT'], writes=[('ps', bc)], mark=(k == 7))
                P.op('dve', lambda e, ba=ba, xs_=xs_, j=j: e.scalar_tensor_tensor(
                    tmpc, ps[:, ba, :], ra[:, j:j + 1], xcs[xs_], ALU.mult, ALU.add),
                    reads=[('ps', ba), ('xc', xs_)], writes=['tmpc'])
                P.op('dve', lambda e, bc=bc, j=j, cb=cb: e.scalar_tensor_tensor(
                    h1[:, j, cb * 512:(cb + 1) * 512], ps[:, bc, :], rc[:, j:j + 1], tmpc, ALU.mult, ALU.add),
                    reads=[('ps', bc), 'tmpc'], writes=[('h1', j)])
                if cb == 3 and moe:
                    P.op('act', lambda e, j=j: e.activation(junkO, h1[:, j, :], AF.Square, accum_out=ssf[:, j:j + 1]),
                         reads=[('h1', j)], writes=['junkO', ('ssf', j)])
                it += 1
            if cb + 2 < 4:
                load_wo(cb + 2)
        P.barrier()

        if moe:
            moe_phase(nc, P, A, AB, ps, psb, h1, R1 + 16384, TOT, cp, identf, identb, iotar, brt_s, ssf, rf, st,
                      wbc_ffn, wr, ccm, rsel, moe_state)

        for j in range(8):
            P.dma('sp', lambda e, j=j: e.dma_start(out=out[j * 128:(j + 1) * 128, :], in_=h1[:, j, :]), 'outs%d' % j,
                  reads=[('h1', j)])
        P.barrier()
        P.ops['sp'].append((None, P._waits('sp', P._deps('sp', (), ())), None, 0))

        sems = {}
        for name in P.sem_names():
            sems[name] = es.enter_context(nc.semaphore("s_" + name))
        block = es.enter_context(nc.Block())

        def run(eng, e):
            for fn, waits, inc, amt in P.ops[eng]:
                for (s_, v) in waits:
                    e.wait_ge(sems[s_], v)
                if fn is None:
                    continue
                ins = fn(e)
                if inc is not None:
                    ins.then_inc(sems[inc], amt)

        @block.tensor
        def _(e):
            run('pe', e)

        @block.scalar
        def _(e):
            run('act', e)

        @block.vector
        def _(e):
            run('dve', e)

        @block.gpsimd
        def _(e):
            run('pool', e)

        @block.sync
        def _(e):
            run('sp', e)
    return nc


NR = 15


def moe_ring(P, AB, ring_fix, base, w_gate, w_up, w_down):
    pers = 8192 + 2048 + 1024 + 1024 + 1024 + 512 + 512 + 1024 + 256 + 256 + 2048 + 512
    late_o = base + pers
    assert late_o + 7 * 1024 <= ring_fix, (late_o, ring_fix)
    offs = [ring_fix + i * 1024 for i in range(8)] + [late_o + i * 1024 for i in range(7)]
    ring = [AB(offs[i], 1024) for i in range(NR)]
    chunks = []
    for ex in range(NE):
        for k in range(16):
            chunks.append((ex, 'gu', k))
        for k in range(8):
            chunks.append((ex, 'd', k))

    def load(ci):
        ex, typ, k = chunks[ci]
        s = ci % NR
        if typ == 'gu':
            P.dma_group('pool', [
                lambda e, s=s, ex=ex, k=k: e.dma_start(out=ring[s][:, 0:1024], in_=w_gate[ex, k * 128:(k + 1) * 128, :]),
                lambda e, s=s, ex=ex, k=k: e.dma_start(out=ring[s][:, 1024:2048], in_=w_up[ex, k * 128:(k + 1) * 128, :])],
                ['rg%da' % s, 'rg%db' % s], writes=[('ring', s)])
        else:
            P.dma('pool', lambda e, s=s, ex=ex, k=k: e.dma_start(out=ring[s], in_=w_down[ex, k * 128:(k + 1) * 128, :]),
                  'rg%da' % s, writes=[('ring', s)])
    return {'ring': ring, 'chunks': chunks, 'load': load, 'late_o': late_o}


def moe_phase(nc, P, A, AB, ps, psb, h1, base, TOT, cp, identf, identb, iotar, brt_s, ssf, rf, st,
              wbc_ffn, wr, ccm, rsel, ms):
    BIG = 1000.0
    ring, chunks, load_chunk = ms['ring'], ms['chunks'], ms['load']
    o = base
    hn_o = o; o += 8192
    xe_o = [o, o + 1024]; o += 2048
    S_o = [o, o + 512]; o += 1024
    ST_o = [o, o + 512]; o += 1024
    sg_o = o; o += 1024
    H_o = o; o += 512
    HT_o = o; o += 512
    Y_o = o; o += 1024
    Wt_o = o; o += 256
    posm_o = o; o += 256
    rs_o = o; o += 2048
    ptm_o = o; o += 512
    assert o == ms['late_o'], (o, ms['late_o'])
    junk3_o = o; o += 1024
    wr_o = o; o += 576
    lg_o = o; o += 288
    sm_o = o; o += 1600
    cc_o = o; o += 960
    ptf_o = o; o += 1024
    mt_o = o; o += 1024
    assert o <= ms['late_o'] + 7 * 1024, o
    hn32 = A(xe_o[0], 2048)
    hnT = A(xe_o[0] + 2048, 2048).rearrange("p (k t) -> p k t", k=16)
    wffn = A(xe_o[0] + 4096, 2048)
    assert xe_o[0] + 6144 <= Wt_o
    junk3 = AB(junk3_o, 1024)

    hn = AB(hn_o, 8192).rearrange("p (j d) -> p j d", j=8)
    xe = [AB(xe_o[i], 1024).rearrange("p (k s) -> p k s", k=16) for i in range(2)]
    Sb = [AB(S_o[i], 512).rearrange("p (j s) -> p j s", j=8) for i in range(2)]
    STb = [AB(ST_o[i], 512) for i in range(2)]
    sg = A(sg_o, 1024)
    Hb = AB(H_o, 512)
    HTb = AB(HT_o, 512).rearrange("p (k s) -> p k s", k=8)
    Yb = AB(Y_o, 1024)
    wrs = A(wr_o, 576).rearrange("p (k n) -> p k n", k=16)
    lg = A(lg_o, 288)
    L3 = lg.rearrange("p (j n) -> p j n", j=8)
    sm = A(sm_o, 1600)
    posm = A(posm_o, 256)
    Wt = A(Wt_o, 256)
    ccb = AB(cc_o, 960)
    rsb_ = AB(rs_o, 2048)
    ptm = AB(ptm_o, 512)
    ptf = A(ptf_o, 1024)
    mts = A(mt_o, 1024)

    P.dma('sp', lambda e: e.dma_start(out=wffn, in_=wbc_ffn[:, :]), 'k10', writes=['wffn'])
    P.dma('sp', lambda e: e.dma_start(out=A(wr_o, 576), in_=wr[:, :]), 'k11', writes=['wrs'])
    P.dma('pool', lambda e: e.dma_start(out=ccb, in_=ccm[:, :]), 'k12', writes=['ccb'])
    P.dma('pool', lambda e: e.dma_start(out=rsb_[0:32, :], in_=rsel[:, :]), 'k13', writes=['rsel'])

    P.op('act', lambda e: e.activation(rf, ssf, AF.Sqrt, bias=EPS, scale=1.0 / 2048),
         reads=[('ssf', j) for j in range(8)], writes=['rf'])
    P.op('dve', lambda e: e.reciprocal(rf, rf), reads=['rf'], writes=['rf'])
    for j in range(8):
        P.op('dve', lambda e, j=j: e.scalar_tensor_tensor(hn32, h1[:, j, :], rf[:, j:j + 1], wffn, ALU.mult, ALU.mult),
             reads=[('h1', j), 'rf', 'wffn'], writes=['hn32'])
        P.op('act', lambda e, j=j: e.copy(hn[:, j, :], hn32), reads=['hn32'], writes=['hn'])
        for g in range(4):
            for kk in range(4):
                k = g * 4 + kk
                P.op('pe', lambda e, g=g, kk=kk, k=k: e.transpose(ps[:, g, kk * 128:(kk + 1) * 128],
                                                                   hn32[:, k * 128:(k + 1) * 128], identf),
                     reads=['hn32', 'const'], writes=[('ps', g)], mark=(kk == 3))
            if g % 2 == 0:
                P.op('act', lambda e, g=g: e.copy(hnT[:, g * 4:g * 4 + 4, :], ps[:, g, :].rearrange("p (k t) -> p k t", k=4)),
                     reads=[('ps', g)], writes=['hnT'])
            else:
                P.op('dve', lambda e, g=g: e.tensor_copy(hnT[:, g * 4:g * 4 + 4, :], ps[:, g, :].rearrange("p (k t) -> p k t", k=4)),
                     reads=[('ps', g)], writes=['hnT'])
        for k in range(16):
            P.op('pe', lambda e, k=k, j=j: e.matmul(ps[:, 4, j * 36:(j + 1) * 36], hnT[:, k, :], wrs[:, k, :],
                                                    start=(k == 0), stop=(k == 15)),
                 reads=['hnT', 'wrs'], writes=[('ps', 4)], mark=(k == 15))
    P.op('dve', lambda e: e.tensor_tensor(lg, ps[:, 4, 0:288], brt_s, ALU.add), reads=[('ps', 4), 'const'], writes=['lg'])

    gmax = sm[:, 0:8]
    gm = sm[:, 8:40].rearrange("p (j g) -> p j g", j=8)
    gd = sm[:, 40:72].rearrange("p (j g) -> p j g", j=8)
    gs = sm[:, 72:80]
    gw = sm[:, 80:88]
    lem = sm[:, 96:352]
    lem4 = lem.rearrange("p (j g x) -> p j g x", j=8, g=4)
    lem3 = lem.rearrange("p (j n) -> p j n", j=8)
    mx = sm[:, 352:416].rearrange("p (j x) -> p j x", j=8)
    sel = sm[:, 416:672]
    sel3 = sel.rearrange("p (j n) -> p j n", j=8)
    pe_ = sm[:, 672:928]
    pe3 = pe_.rearrange("p (j n) -> p j n", j=8)
    e2 = sm[:, 928:936]
    scl = sm[:, 936:944]
    Mbf = A(sm_o + 944, 128).bitcast(BF16).rearrange("p (j n) -> p j n", j=8)
    pos_t = sm[:, 1072:1328]
    iop = cp[:, C_IOP:C_IOP + 1]

    def b3(ap2, n):
        return ap2.rearrange("p (j o) -> p j o", o=1).to_broadcast([128, 8, n])

    def rt(eng, fn):
        P.op(eng, fn, reads=['rt', 'lg'], writes=['rt'])

    rt('dve', lambda e: e.tensor_reduce(gmax, L3[:, :, 0:4], AX.X, ALU.max))
    rt('dve', lambda e: e.tensor_tensor(gm, L3[:, :, 0:4], b3(gmax, 4), ALU.is_ge))
    rt('dve', lambda e: e.tensor_tensor(gd, L3[:, :, 0:4], b3(gmax, 4), ALU.subtract))
    rt('act', lambda e: e.activation(gd, gd, AF.Exp))
    rt('dve', lambda e: e.tensor_reduce(gs, gd, AX.X, ALU.add))
    rt('dve', lambda e: e.reciprocal(gw, gs))
    rt('dve', lambda e: e.tensor_scalar(gm, gm, BIG, -BIG, ALU.mult, ALU.add))
    rt('dve', lambda e: e.tensor_tensor(
        lem4, L3[:, :, 4:36].rearrange("p j (g x) -> p j g x", g=4),
        gm.rearrange("p j (g o) -> p j g o", o=1).to_broadcast([128, 8, 4, 8]), ALU.add))
    for j in range(8):
        rt('dve', lambda e, j=j: e.max(out=mx[:, j, :], in_=lem3[:, j, :]))
    rt('dve', lambda e: e.tensor_tensor(sel3, lem3, mx[:, :, 1:2].to_broadcast([128, 8, 32]), ALU.is_ge))
    rt('dve', lambda e: e.tensor_tensor(pe3, lem3, mx[:, :, 0:1].to_broadcast([128, 8, 32]), ALU.subtract))
    rt('act', lambda e: e.activation(pe_, pe_, AF.Exp))
    rt('dve', lambda e: e.tensor_tensor(pe_, pe_, sel, ALU.mult))
    rt('dve', lambda e: e.tensor_tensor(e2.rearrange("p (j o) -> p j o", o=1), mx[:, :, 1:2], mx[:, :, 0:1], ALU.subtract))
    rt('act', lambda e: e.activation(e2, e2, AF.Exp))
    rt('dve', lambda e: e.tensor_scalar(e2, e2, 1.0, None, ALU.add))
    rt('dve', lambda e: e.reciprocal(e2, e2))
    rt('dve', lambda e: e.tensor_tensor(scl, gw, e2, ALU.mult))
    rt('dve', lambda e: e.tensor_tensor(Wt.rearrange("p (j n) -> p j n", j=8), pe3, b3(scl, 32), ALU.mult))
    rt('dve', lambda e: e.tensor_copy(Mbf, sel3))

    for j in range(8):
        for n2 in range(2):
            P.op('pe', lambda e, j=j, n2=n2: e.matmul(
                ps[0:32, n2, :], Mbf[:, j, :], ccb[:, (7 - j) * 128 + n2 * 512:(7 - j) * 128 + n2 * 512 + 512],
                start=(j == 0), stop=(j == 7)),
                reads=['rt', 'ccb'], writes=[('ps', n2)], mark=(j == 7))
    for j in range(8):
        b = 2 + j // 4
        P.op('pe', lambda e, j=j, b=b: e.transpose(ps[0:32, b, (j % 4) * 128:(j % 4 + 1) * 128], sel3[:, j, :], identf),
             reads=['rt', 'const'], writes=[('ps', b)], mark=(j % 4 == 3))
    for n2 in range(2):
        P.op('act', lambda e, n2=n2: e.copy(mts[0:32, n2 * 512:(n2 + 1) * 512], ps[0:32, 2 + n2, :]),
             reads=[('ps', 2 + n2)], writes=['mts'])
        P.op('dve', lambda e, n2=n2: e.scalar_tensor_tensor(
            ptf[0:32, n2 * 512:(n2 + 1) * 512], ps[0:32, n2, :], 1.0, mts[0:32, n2 * 512:(n2 + 1) * 512], ALU.add, ALU.mult),
            reads=[('ps', n2), 'mts'], writes=['ptf'])
    P.op('dve', lambda e: e.tensor_scalar(ptf[0:32, :], ptf[0:32, :], -1.0, None, ALU.add), reads=['ptf'], writes=['ptf'])
    P.op('act', lambda e: e.copy(ptm[0:32, :], ptf[0:32, :]), reads=['ptf'], writes=['ptm'])
    for j in range(8):
        P.op('pe', lambda e, j=j: e.transpose(ps[:, 4, j * 32:(j + 1) * 32], ptf[0:32, j * 128:(j + 1) * 128], identf[0:32, 0:32]),
             reads=['ptf', 'const'], writes=[('ps', 4)], mark=(j == 7))
    P.op('dve', lambda e: e.tensor_copy(posm, ps[:, 4, 0:256]), reads=[('ps', 4)], writes=['posm'])
    posm3 = posm.rearrange("p (j n) -> p j n", j=8)
    Wt3 = Wt.rearrange("p (j n) -> p j n", j=8)
    P.barrier()

    for ci in range(8, NR):
        load_chunk(ci)
    ci = 0
    rot = 0
    def build_S(ex):
        sl = ex % 2
        for j in range(8):
            P.op('dve', lambda e, j=j, ex=ex, sl=sl: e.tensor_scalar(Sb[sl][:, j, :], iotar, posm3[:, j, ex:ex + 1], None, ALU.is_equal),
                 reads=['posm', 'const'], writes=[('S', sl)])
        for n2 in range(2):
            b = 6 + n2
            P.op('pe', lambda e, n2=n2, b=b, ex=ex: e.matmul(ps[:, b, :], rsb_[0:32, ex * 128:(ex + 1) * 128],
                                                             ptm[0:32, n2 * 512:(n2 + 1) * 512], start=True, stop=True),
                 reads=['ptm', 'rsel'], writes=[('ps', b)])
            P.op('dve', lambda e, n2=n2, b=b, sl=sl: e.tensor_scalar(STb[sl][:, n2 * 512:(n2 + 1) * 512], ps[:, b, :], iop, None, ALU.is_equal),
                 reads=[('ps', b), 'const'], writes=[('ST', sl)])

    build_S(0)
    for ex in range(NE):
        sl = ex % 2
        for g in range(4):
            b = g % 2
            for kk in range(4):
                dk = g * 4 + kk
                for j in range(8):
                    P.op('pe', lambda e, b=b, kk=kk, dk=dk, j=j, sl=sl: e.matmul(
                        ps[:, b, kk * 128:(kk + 1) * 128], hn[:, j, dk * 128:(dk + 1) * 128], Sb[sl][:, j, :],
                        start=(j == 0), stop=(j == 7)),
                        reads=['hn', ('S', sl)], writes=[('ps', b)], mark=(kk == 3 and j == 7))
            P.op('act', lambda e, b=b, g=g, sl=sl: e.copy(xe[sl][:, g * 4:g * 4 + 4, :], ps[:, b, :].rearrange("p (k s) -> p k s", k=4)),
                 reads=[('ps', b)], writes=[('xe', sl)])
        for k in range(16):
            s = ci % NR
            for n4 in range(4):
                P.op('pe', lambda e, k=k, n4=n4, s=s, sl=sl: e.matmul(
                    ps[:, 2 + n4, :], xe[sl][:, k, :], ring[s][:, n4 * 512:(n4 + 1) * 512], start=(k == 0), stop=(k == 15)),
                    reads=[('xe', sl), ('ring', s)], writes=[('ps', 2 + n4)], mark=(n4 == 3))
            if ci + NR < len(chunks):
                load_chunk(ci + NR)
            ci += 1
        for n2 in range(2):
            P.op('act', lambda e, n2=n2: e.activation(sg[:, n2 * 512:(n2 + 1) * 512], ps[:, 2 + n2, :], AF.Silu),
                 reads=[('ps', 2 + n2)], writes=['sg'])
            P.op('dve', lambda e, n2=n2: e.tensor_tensor(Hb[:, n2 * 512:(n2 + 1) * 512], sg[:, n2 * 512:(n2 + 1) * 512], ps[:, 4 + n2, :], ALU.mult),
                 reads=[('ps', 4 + n2), 'sg'], writes=['H'])
        for k in range(8):
            P.op('pe', lambda e, k=k: e.transpose(psb(6)[:, k * 128:(k + 1) * 128], Hb[:, k * 128:(k + 1) * 128], identb),
                 reads=['H', 'const'], writes=[('ps', 6)], mark=(k == 7))
        P.op('act', lambda e: e.copy(HTb, psb(6).rearrange("p (k s) -> p k s", k=8)), reads=[('ps', 6)], writes=['HT'])
        for k in range(8):
            s = ci % NR
            for n4 in range(4):
                P.op('pe', lambda e, k=k, n4=n4, s=s: e.matmul(
                    ps[:, 2 + n4, :], HTb[:, k, :], ring[s][:, n4 * 512:(n4 + 1) * 512], start=(k == 0), stop=(k == 7)),
                    reads=['HT', ('ring', s)], writes=[('ps', 2 + n4)], mark=(n4 == 3))
            if ci + NR < len(chunks):
                load_chunk(ci + NR)
            ci += 1
        if ex + 1 < NE:
            build_S(ex + 1)
        for n4 in range(4):
            if n4 % 2 == 0:
                P.op('act', lambda e, n4=n4: e.copy(Yb[:, n4 * 512:(n4 + 1) * 512], ps[:, 2 + n4, :]),
                     reads=[('ps', 2 + n4)], writes=['Y'])
            else:
                P.op('dve', lambda e, n4=n4: e.tensor_copy(Yb[:, n4 * 512:(n4 + 1) * 512], ps[:, 2 + n4, :]),
                     reads=[('ps', 2 + n4)], writes=['Y'])
        sbanks = [0, 1, 6, 7]
        for j in range(8):
            for n4 in range(4):
                b = sbanks[rot % 4]
                rot += 1
                P.op('pe', lambda e, b=b, j=j, n4=n4, sl=sl: e.matmul(
                    ps[:, b, :], STb[sl][:, j * 128:(j + 1) * 128], Yb[:, n4 * 512:(n4 + 1) * 512], start=True, stop=True),
                    reads=[('ST', sl), 'Y'], writes=[('ps', b)])
                P.op('dve', lambda e, b=b, j=j, n4=n4, ex=ex: e.scalar_tensor_tensor(
                    h1[:, j, n4 * 512:(n4 + 1) * 512], ps[:, b, :], Wt3[:, j, ex:ex + 1], h1[:, j, n4 * 512:(n4 + 1) * 512],
                    ALU.mult, ALU.add),
                    reads=[('ps', b), 'rt'], writes=[('h1', j)])
    P.barrier()


def _bias_tables(rel_bias, hf):
    nh = rel_bias.shape[0]
    c = np.arange(64)
    col_start = np.clip(c - 8, 0, 48)
    cmask = (c[None, :] >= col_start[:, None]) & (c[None, :] < col_start[:, None] + 16)
    dc = np.clip(c[None, :] - c[:, None], -15, 15) + 15
    tabs = np.full((NSLOT, 2, 64, nh, 2, 64), NEG, np.float32)
    glob = {'A': 8 if hf == 0 else 6, 'B': 9 if hf == 0 else 7}
    for j in range(8):
        gq = 8 * hf + j
        for si, kc in enumerate(SLOTS[j]):
            gc = glob[kc] if isinstance(kc, str) else 8 * hf + kc
            t = tabs[SLOT_BASE[j] + si]
            for rqp in range(2):
                rq = 2 * gq + rqp
                start = min(max(rq - 4, 0), 24)
                for rkp in range(2):
                    rk = 2 * gc + rkp
                    if not (start <= rk < start + 8):
                        continue
                    dr = rk - rq + 7
                    blk = rel_bias[:, dr, :][:, dc]
                    blk = np.where(cmask[None], blk, np.float32(NEG))
                    t[rkp, :, :, rqp, :] = blk.transpose(2, 0, 1)
    tabs = tabs[:, :, :, HORDER]
    return tabs.reshape(NSLOT, 128, nh * 128)


def _prep(x, meta_tokens, mix_norm_w, w_in, q_norm_w, k_norm_w, rel_bias, meta_bias, conv_w,
          attn_out_norm_w, conv_out_norm_w, w_out, ffn_norm_w, w_router_group, b_router_group,
          w_router_expert, b_router_expert, w_gate, w_up, w_down, moe=True):
    f = lambda a: np.ascontiguousarray(np.asarray(a), dtype=np.float32)
    x = f(x); meta = f(meta_tokens)
    p = np.arange(128)
    cpack = np.zeros((128, NCP), np.float32)
    cpack[:, C_WQ] = f(q_norm_w)[0][p % 64]
    cpack[:, C_WK] = f(k_norm_w)[0][p % 64]
    cw = f(conv_w)[0]
    for ct in range(8):
        for tap in range(3):
            cpack[:, C_CW + ct * 3 + tap] = cw[tap, ct * 128:(ct + 1) * 128]
        cpack[:, C_WC + ct] = f(conv_out_norm_w)[0][ct * 128:(ct + 1) * 128]
    cpack[:, C_IOP] = p
    cpack[:, C_ONE] = 1.0
    rep = lambda v: np.ascontiguousarray(np.broadcast_to(f(v).reshape(1, -1), (128, f(v).size)))
    bcat = np.concatenate([f(b_router_group)[0], f(b_router_expert)[0]])
    wcat = np.concatenate([f(w_router_group)[0], f(w_router_expert)[0]], axis=1)
    cmat = np.zeros((128, 384), np.float32)
    cmat[:, 0:128] = np.eye(128)
    cmat[:, 128:256] = np.kron(np.eye(2), np.ones((64, 64)))
    cmat[:, 256:384] = np.arange(128)[None, :]
    ccm = np.zeros((128, 1920), np.float32)
    ccm[:, 896:1024] = (p[:, None] < p[None, :])
    ccm[:, 1024:] = 1.0
    rsel = np.zeros((32, NE, 128), np.float32)
    rsel[np.arange(32), np.arange(32), :] = 1.0
    mb = f(meta_bias)[0]
    tabm = np.ascontiguousarray(np.broadcast_to(mb.T[:, HORDER, None], (16, 16, 128))).reshape(16, 2048)
    shared = {
        "w_in": f(w_in)[0], "w_out": f(w_out)[0], "tabm": tabm, "cpack": cpack,
        "wbc_mix": rep(mix_norm_w), "wbc_attn": rep(attn_out_norm_w), "wbc_ffn": rep(ffn_norm_w),
        "brt": rep(np.tile(bcat, 8)),
        "wr": np.ascontiguousarray(wcat.reshape(16, 128, 36).transpose(1, 0, 2)).reshape(128, 576),
        "cmat": cmat, "ccm": ccm, "rsel": rsel.reshape(32, NE * 128),
    }
    if moe:
        shared.update({"w_gate": f(w_gate)[0], "w_up": f(w_up)[0], "w_down": f(w_down)[0]})
    tabs = [_bias_tables(f(rel_bias)[0], hf) for hf in range(2)]
    in_maps = []
    for c in range(8):
        b, hf = c // 2, c % 2
        xg = np.zeros((288, 2048), np.float32)
        if hf == 0:
            xg[0:256] = x[b, 1024:1280]
            xg[272] = meta[15]
            xg[273] = x[b, 1024]
        else:
            xg[0:256] = x[b, 768:1024]
            xg[272] = x[b, 1023]
        xg[256:272] = meta
        m = dict(shared)
        m["xo"] = np.ascontiguousarray(x[b, hf * 1024:(hf + 1) * 1024])
        m["xg"] = xg
        m["tab"] = tabs[hf]
        in_maps.append(m)
    return in_maps


_NC_CACHE = {}


def kernel(moe=True, stage=9, **inputs):
    in_maps = _prep(moe=moe, **inputs)
    if (moe, stage) not in _NC_CACHE:
        _NC_CACHE[(moe, stage)] = build(moe=moe, stage=stage)
    res = run_bass_kernel_spmd(_NC_CACHE[(moe, stage)], in_maps, core_ids=list(range(8)))
    out = np.zeros((4, 2048, 2048), np.float32)
    for c in range(8):
        b, hf = c // 2, c % 2
        out[b, hf * 1024:(hf + 1) * 1024] = res.results[c]["out"]
    return out
```

```python
from contextlib import ExitStack

import numpy as np
import concourse.bass as bass
import concourse.mybir as mybir
from concourse.bass_utils import run_bass_kernel_spmd

F32 = mybir.dt.float32
BF16 = mybir.dt.bfloat16
AF = mybir.ActivationFunctionType
ALU = mybir.AluOpType
AX = mybir.AxisListType

EPS = 1e-6
NEG = -30000.0
NE = 32
SLOTS = [[0, 1, 2, 3, 'A', 'B'], [0, 1, 2, 3, 'B'], [0, 1, 2, 3, 4], [1, 2, 3, 4, 5],
         [2, 3, 4, 5, 6], [3, 4, 5, 6, 7], [4, 5, 6, 7, 'A'], [4, 5, 6, 7, 'A', 'B']]
SLOT_BASE = np.cumsum([0] + [len(s) for s in SLOTS]).tolist()
NSLOT = SLOT_BASE[-1]
ENG = ['pe', 'act', 'dve', 'pool', 'sp']
C_WQ, C_WK, C_CW, C_WC, C_IOP, C_ONE = 0, 1, 2, 26, 34, 35
NCP = 36
HORDER = [8 * (u // 2) + 2 * i + (u % 2) for u in range(4) for i in range(4)]


class Prog:
    def __init__(self):
        self.ops = {e: [] for e in ENG}
        self.cnt = {e: 0 for e in ENG}
        self.seen = {e: {} for e in ENG}
        self.lastw = {}
        self.readers = {}
        self.dsem = {}
        self.pending = {e: [] for e in ENG}

    def _deps(self, eng, reads, writes):
        deps = list(self.pending[eng])
        self.pending[eng] = []
        for k in reads:
            deps += self.lastw.get(k, [])
        for k in writes:
            deps += self.lastw.get(k, [])
            deps += self.readers.get(k, [])
        return deps

    def _waits(self, eng, deps):
        waits = []
        best = {}
        for (s, v) in deps:
            if s == eng and eng == 'pe':
                continue
            if self.seen[eng].get(s, 0) >= v:
                continue
            best[s] = max(best.get(s, 0), v)
        for s, v in best.items():
            self.seen[eng][s] = v
            waits.append((s, v))
        return waits

    def _record(self, evs, reads, writes):
        for k in writes:
            self.lastw[k] = list(evs)
            self.readers[k] = []
        for k in reads:
            self.readers.setdefault(k, []).extend(evs)

    def op(self, eng, fn, reads=(), writes=(), mark=True):
        assert mark or eng == 'pe'
        waits = self._waits(eng, self._deps(eng, reads, writes))
        if mark:
            self.cnt[eng] += 1
            ev = (eng, self.cnt[eng])
        else:
            ev = (eng, self.cnt[eng] + 1)
        self.ops[eng].append((fn, waits, eng if mark else None, 1))
        self._record([ev], reads, writes)
        return ev

    def dma(self, q, fn, sem, reads=(), writes=()):
        return self.dma_group(q, [fn], [sem], reads, writes)

    def dma_group(self, q, fns, sems, reads=(), writes=()):
        deps = self._deps(q, reads, writes)
        for sem in sems:
            cur = self.dsem.get(sem, 0)
            if cur > 0:
                deps.append((sem, cur))
        waits = self._waits(q, deps)
        evs = []
        for i, (fn, sem) in enumerate(zip(fns, sems)):
            self.dsem[sem] = self.dsem.get(sem, 0) + 16
            evs.append((sem, self.dsem[sem]))
            self.ops[q].append((fn, waits if i == 0 else [], sem, 16))
        self._record(evs, reads, writes)
        return evs

    def barrier(self):
        evs = [(e, self.cnt[e]) for e in ENG if self.cnt[e] > 0]
        evs += [(s, v) for s, v in self.dsem.items()]
        for e in ENG:
            self.pending[e] = list(evs)
        self.lastw = {}
        self.readers = {}

    def sem_names(self):
        return list(ENG) + list(self.dsem.keys())


def build(moe=True, stage=9):
    nc = bass.Bass("TRN2", target_bir_lowering=False)

    def din(name, shape):
        return nc.dram_tensor(name, shape, F32, kind="ExternalInput").ap()

    xo = din("xo", [1024, 2048])
    xg = din("xg", [288, 2048])
    w_in = din("w_in", [2048, 6144])
    w_out = din("w_out", [2048, 2048])
    tab = din("tab", [NSLOT, 128, 2048])
    tabm = din("tabm", [16, 2048])
    cpack = din("cpack", [128, NCP])
    wbc_mix = din("wbc_mix", [128, 2048])
    wbc_attn = din("wbc_attn", [128, 1024])
    wbc_ffn = din("wbc_ffn", [128, 2048])
    brt = din("brt", [128, 288])
    wr = din("wr", [128, 16 * 36])
    cmat = din("cmat", [128, 128 * 3])
    ccm = din("ccm", [128, 1920])
    rsel = din("rsel", [32, NE * 128])
    if moe:
        w_gate = din("w_gate", [NE, 2048, 1024])
        w_up = din("w_up", [NE, 2048, 1024])
        w_down = din("w_down", [NE, 1024, 2048])
    out = nc.dram_tensor("out", [1024, 2048], F32, kind="ExternalOutput").ap()

    P = Prog()
    TOT = 53000
    with ExitStack() as es:
        arena = es.enter_context(nc.sbuf_tensor("arena", [128, TOT], F32))
        ps = es.enter_context(nc.psum_tensor("ps", [128, 8, 512], F32))

        def A(off, n):
            assert off + n <= TOT, (off, n)
            return arena[:, off:off + n]

        def AB(off, n):
            return A(off, n).bitcast(BF16)

        def psb(b):
            return ps[:, b, :].bitcast(BF16)

        o = 0
        cp = A(o, NCP); o += NCP
        identf = A(o, 128); o += 128
        bones = A(o, 128); o += 128
        iotar = A(o, 128); o += 128
        identb = AB(o, 64); o += 64
        st = A(o, 160); o += 160
        brt_s = A(o, 288); o += 288
        wq8 = A(o, 1); o += 1
        CONST_END = 2560
        assert o <= CONST_END
        ss, rstd = st[:, 0:16], st[:, 16:32]
        ssa, ra, rc = st[:, 32:40], st[:, 40:48], st[:, 48:56]
        ssf, rf = st[:, 56:64], st[:, 64:72]
        rec4 = st[:, 72:88]

        P.dma('sp', lambda e: e.dma_start(out=cp, in_=cpack[:, :]), 'k1', writes=['const'])
        P.dma('sp', lambda e: e.dma_start(out=identf, in_=cmat[:, 0:128]), 'k2', writes=['const'])
        P.dma('sp', lambda e: e.dma_start(out=bones, in_=cmat[:, 128:256]), 'k3', writes=['const'])
        P.dma('sp', lambda e: e.dma_start(out=iotar, in_=cmat[:, 256:384]), 'k4', writes=['const'])
        P.dma('sp', lambda e: e.dma_start(out=brt_s, in_=brt[:, :]), 'k5', writes=['const'])
        P.dma('pool', lambda e: e.dma_start(out=identb, in_=cmat[:, 0:128]), 'k6', writes=['const'])

        R1 = CONST_END
        o = R1
        qT_o = o; o += 4096
        kT_o = o; o += 5248
        V_o = o; o += 5808
        U_o = o; o += 8208
        R2 = o
        cT_o = o; o += 4096
        R3 = o
        xTo_o = o; o += 8192
        xTg_o = o; o += 2304
        wb_o = [o, o + 4096]; o += 8192
        assert o <= TOT, o
        qT = AB(qT_o, 4096).rearrange("p (c t) -> p c t", c=8)
        kT = AB(kT_o, 5248).rearrange("p (c t) -> p c t", c=8)
        Vv = AB(V_o, 5808).rearrange("p (c h d) -> p c h d", c=11, h=16)
        U = A(U_o, 8208).rearrange("p (c t) -> p c t", c=8)
        cT = AB(cT_o, 4096).rearrange("p (c t) -> p c t", c=8)
        xTo = AB(xTo_o, 8192).rearrange("p (k t) -> p k t", k=16)
        xTg = AB(xTg_o, 2304).rearrange("p (k t) -> p k t", k=16)
        wblk = [AB(wb_o[i], 4096).rearrange("p (k c) -> p k c", k=16) for i in range(2)]
        xst = [A(U_o + i * 2048, 2048) for i in range(2)] + [A(cT_o + 1024, 2048)]
        xsb = [AB(U_o + 4096 + i * 1024, 1024) for i in range(2)]
        wmix = A(U_o + 6144, 2048)
        junk = AB(cT_o, 1024)

        P.dma('sp', lambda e: e.dma_start(out=wmix, in_=wbc_mix[:, :]), 'k7', writes=['wmix'])
        P.op('dve', lambda e: e.tensor_scalar(wq8, cp[:, C_WQ:C_WQ + 1], 0.125, None, ALU.mult),
             reads=['const'], writes=['wq8'])

        blocks_enabled = stage >= 2
        blocks = [('q', 0), ('q', 1), ('k', 0), ('k', 1), ('v', 0), ('v', 1),
                  ('C', 0), ('hc', 0), ('C', 1), ('hc', 1), ('B', 0), ('B', 1)]
        colbase = {'q': 0, 'k': 1024, 'v': 2048, 'B': 3072, 'C': 4096, 'hc': 5120}
        w_in_v = w_in.rearrange("(k p) c -> p k c", p=128)

        def load_wblk(bi):
            typ, half = blocks[bi]
            c0 = colbase[typ] + half * 512
            s = bi % 2
            P.dma_group('pool', [lambda e, s=s, c0=c0, q4=q4: e.dma_start(
                out=wblk[s][:, q4 * 4:(q4 + 1) * 4, :], in_=w_in_v[:, q4 * 4:(q4 + 1) * 4, c0:c0 + 512]) for q4 in range(4)],
                ['wb%d_%d' % (s, q4) for q4 in range(4)], writes=[('wblk', s)])

        if blocks_enabled:
            load_wblk(0)
            load_wblk(1)
        tiles = [(xo, j * 128, 128, xTo, j * 128) for j in range(8)]
        tiles += [(xg, 0, 128, xTg, 0), (xg, 128, 128, xTg, 128), (xg, 256, 32, xTg, 256)]
        for t, (src, r0, R, dst, c0) in enumerate(tiles):
            s = t % 2
            s3 = t % 3
            P.dma('sp', lambda e, s3=s3, src=src, r0=r0, R=R: e.dma_start(out=xst[s3][:R, :], in_=src[r0:r0 + R, :]),
                  'xld%d' % s3, writes=[('xst', s3)])
            P.op('act', lambda e, s3=s3, R=R, t=t: e.activation(junk[:R, :], xst[s3][:R, :], AF.Square,
                                                                accum_out=ss[:R, t:t + 1]),
                 reads=[('xst', s3)], writes=['junk', ('ss', t)])
            P.op('act', lambda e, R=R, t=t: e.activation(rstd[:R, t:t + 1], ss[:R, t:t + 1], AF.Sqrt, bias=EPS, scale=1.0 / 2048),
                 reads=[('ss', t)], writes=[('rstd', t)])
            P.op('dve', lambda e, R=R, t=t: e.reciprocal(rstd[:R, t:t + 1], rstd[:R, t:t + 1]),
                 reads=[('rstd', t)], writes=[('rstd', t)])
            P.op('dve', lambda e, s=s, s3=s3, R=R, t=t: e.scalar_tensor_tensor(xsb[s][:R, :], xst[s3][:R, :], rstd[:R, t:t + 1],
                                                                               wmix[:R, :], ALU.mult, ALU.mult),
                 reads=[('xst', s3), ('rstd', t), 'wmix'], writes=[('xsb', s)])
            for hb in range(2):
                bank = (t % 2) * 2 + hb
                for kk in range(8):
                    k = hb * 8 + kk
                    P.op('pe', lambda e, s=s, R=R, k=k, kk=kk, bank=bank: e.transpose(
                        psb(bank)[:, kk * 128:kk * 128 + R], xsb[s][:R, k * 128:(k + 1) * 128], identb[:R, :R]),
                        reads=[('xsb', s), 'const'], writes=[('ps', bank)], mark=(kk == 7))
                src_ps = psb(bank).rearrange("p (k t) -> p k t", k=8)[:, :, 0:R]
                dst_ap = dst[:, hb * 8:hb * 8 + 8, c0:c0 + R]
                if hb == 0:
                    P.op('act', lambda e, a=dst_ap, b=src_ps: e.copy(a, b), reads=[('ps', bank)], writes=['xT'])
                else:
                    P.op('dve', lambda e, a=dst_ap, b=src_ps: e.tensor_copy(a, b), reads=[('ps', bank)], writes=['xT'])
        P.barrier()

        P.op('dve', lambda e: e.memset(Vv[:, :, :, 64:66], 1.0), writes=['V'])
        tl = wb_o[1] + 4096
        sqb = [A(tl, 512), A(tl + 512, 512)]
        rsb = [A(tl + 1024, 512), A(tl + 1536, 512)]
        tb = [A(tl + 2048, 512), A(tl + 2560, 512)]
        assert tl + 3072 <= TOT, tl
        bankrot = [0]
        dpe = []
        srot = [0]
        urot = [0]

        def nextbank():
            b = bankrot[0] % 5
            bankrot[0] += 1
            return b

        for bi, (typ, half) in enumerate(blocks if blocks_enabled else []):
            s = bi % 2
            wb = wblk[s]
            if typ == 'v':
                vt = [(xTo, j * 128, 128, j) for j in range(8)] + [(xTg, 0, 128, 8), (xTg, 128, 128, 9), (xTg, 256, 32, 10)]
                for (xt, c0, R, ch) in vt:
                    b = nextbank()
                    for k in range(16):
                        P.op('pe', lambda e, b=b, xt=xt, c0=c0, R=R, k=k, wb=wb: e.matmul(
                            ps[:R, b, :], xt[:, k, c0:c0 + R], wb[:, k, :], start=(k == 0), stop=(k == 15)),
                            reads=[('wblk', s), 'xT'], writes=[('ps', b)], mark=(k == 15))
                    while dpe:
                        dpe.pop(0)()
                    P.op('act', lambda e, b=b, R=R, ch=ch, half=half: e.copy(
                        Vv[:R, ch, half * 8:half * 8 + 8, 0:64], ps[:R, b, :].rearrange("p (h d) -> p h d", h=8)),
                        reads=[('ps', b)], writes=['V'])
            else:
                for ct in range(4):
                    gct = half * 4 + ct
                    groups = [(xTo, 0, 512, 0), (xTo, 512, 512, 512)]
                    if typ == 'k':
                        groups.append((xTg, 0, 288, 1024))
                    if typ in ('C', 'hc'):
                        groups.append((xTg, 272, 2, -1))
                    for (xt, c0, N, d0) in groups:
                        b = nextbank()
                        for k in range(16):
                            P.op('pe', lambda e, b=b, xt=xt, c0=c0, N=N, k=k, wb=wb, ct=ct: e.matmul(
                                ps[:, b, 0:N], wb[:, k, ct * 128:(ct + 1) * 128], xt[:, k, c0:c0 + N],
                                start=(k == 0), stop=(k == 15)),
                                reads=[('wblk', s), 'xT'], writes=[('ps', b)], mark=(k == 15))
                        while dpe:
                            dpe.pop(0)()
                        if typ in ('q', 'k'):
                            u = urot[0] % 2
                            urot[0] += 1
                            sb = 5 + (srot[0] % 2)
                            srot[0] += 1
                            P.op('act', lambda e, b=b, N=N, u=u: e.activation(sqb[u][:, 0:N], ps[:, b, 0:N], AF.Square),
                                 reads=[('ps', b)], writes=[('sq', u)])
                            dstT = qT if typ == 'q' else kT
                            wcol = wq8 if typ == 'q' else cp[:, C_WK:C_WK + 1]

                            def post(sb=sb, N=N, u=u, b=b, dstT=dstT, wcol=wcol, gct=gct, d0=d0):
                                P.op('pe', lambda e: e.matmul(ps[:, sb, 0:N], bones, sqb[u][:, 0:N], start=True, stop=True),
                                     reads=[('sq', u), 'const'], writes=[('ps', sb)])
                                P.op('act', lambda e: e.activation(rsb[u][:, 0:N], ps[:, sb, 0:N], AF.Sqrt,
                                                                   bias=EPS, scale=1.0 / 64),
                                     reads=[('ps', sb)], writes=[('rs', u)])
                                P.op('dve', lambda e: e.reciprocal(rsb[u][:, 0:N], rsb[u][:, 0:N]),
                                     reads=[('rs', u)], writes=[('rs', u)])
                                P.op('dve', lambda e: e.scalar_tensor_tensor(
                                    dstT[:, gct, d0:d0 + N], ps[:, b, 0:N], wcol, rsb[u][:, 0:N], ALU.mult, ALU.mult),
                                    reads=[('ps', b), ('rs', u), 'wq8', 'const'], writes=['qkT'])
                            dpe.append(post)
                        elif typ == 'C':
                            if d0 >= 0:
                                P.op('act', lambda e, b=b, gct=gct, d0=d0: e.copy(U[:, gct, 1 + d0:1 + d0 + 512], ps[:, b, :]),
                                     reads=[('ps', b)], writes=[('U', gct)])
                            else:
                                P.op('act', lambda e, b=b, gct=gct: e.copy(U[:, gct, 0:1], ps[:, b, 0:1]),
                                     reads=[('ps', b)], writes=[('U', gct)])
                                P.op('act', lambda e, b=b, gct=gct: e.copy(U[:, gct, 1025:1026], ps[:, b, 1:2]),
                                     reads=[('ps', b)], writes=[('U', gct)])
                        elif typ == 'hc':
                            if d0 >= 0:
                                P.op('dve', lambda e, b=b, gct=gct, d0=d0: e.tensor_tensor(
                                    U[:, gct, 1 + d0:1 + d0 + 512], U[:, gct, 1 + d0:1 + d0 + 512], ps[:, b, :], ALU.mult),
                                    reads=[('ps', b), ('U', gct)], writes=[('U', gct)])
                            else:
                                P.op('dve', lambda e, b=b, gct=gct: e.tensor_tensor(
                                    U[:, gct, 0:1], U[:, gct, 0:1], ps[:, b, 0:1], ALU.mult),
                                    reads=[('ps', b), ('U', gct)], writes=[('U', gct)])
                                P.op('dve', lambda e, b=b, gct=gct: e.tensor_tensor(
                                    U[:, gct, 1025:1026], U[:, gct, 1025:1026], ps[:, b, 1:2], ALU.mult),
                                    reads=[('ps', b), ('U', gct)], writes=[('U', gct)])
                        else:
                            u = urot[0] % 2
                            urot[0] += 1
                            n0 = d0
                            cw = lambda tap, gct=gct: cp[:, C_CW + gct * 3 + tap:C_CW + gct * 3 + tap + 1]
                            P.op('dve', lambda e, u=u, gct=gct, n0=n0, cw=cw: e.tensor_scalar(
                                tb[u], U[:, gct, n0:n0 + 512], cw(0), None, ALU.mult),
                                reads=[('U', gct), 'const'], writes=[('tb', u)])
                            P.op('dve', lambda e, u=u, gct=gct, n0=n0, cw=cw: e.scalar_tensor_tensor(
                                tb[u], U[:, gct, n0 + 1:n0 + 513], cw(1), tb[u], ALU.mult, ALU.add),
                                reads=[('U', gct), ('tb', u)], writes=[('tb', u)])
                            P.op('dve', lambda e, u=u, gct=gct, n0=n0, cw=cw: e.scalar_tensor_tensor(
                                tb[u], U[:, gct, n0 + 2:n0 + 514], cw(2), tb[u], ALU.mult, ALU.add),
                                reads=[('U', gct), ('tb', u)], writes=[('tb', u)])
                            P.op('dve', lambda e, u=u, gct=gct, b=b: e.scalar_tensor_tensor(
                                tb[u], ps[:, b, :], cp[:, C_WC + gct:C_WC + gct + 1], tb[u], ALU.mult, ALU.mult),
                                reads=[('ps', b), ('tb', u)], writes=[('tb', u)])
                            P.op('act', lambda e, u=u, gct=gct, n0=n0: e.copy(cT[:, gct, n0:n0 + 512], tb[u]),
                                 reads=[('tb', u)], writes=['cT'])
                            P.op('act', lambda e, u=u: e.activation(sqb[u], tb[u], AF.Square),
                                 reads=[('tb', u)], writes=[('sq', u)])
                            def postb(u=u, n0=n0, gct=gct):
                              for tt in range(4):
                                j = n0 // 128 + tt
                                P.op('pe', lambda e, u=u, tt=tt, j=j, gct=gct: e.matmul(
                                    ps[:, 7, j:j + 1], sqb[u][:, tt * 128:(tt + 1) * 128], cp[:, C_ONE:C_ONE + 1],
                                    start=(gct == 0 and j == 0), stop=(gct == 7), skip_group_check=True),
                                    reads=[('sq', u), 'const'], writes=[('ps', 7)], mark=(tt == 3))
                            dpe.append(postb)
            if bi + 2 < len(blocks):
                load_wblk(bi + 2)
        while dpe:
            dpe.pop(0)()
        P.op('act', lambda e: e.activation(rc, ps[:, 7, 0:8], AF.Sqrt, bias=EPS, scale=1.0 / 1024),
             reads=[('ps', 7)], writes=['rc'])
        P.op('dve', lambda e: e.reciprocal(rc, rc), reads=['rc'], writes=['rc'])
        P.barrier()

        o = R3
        aT_o = o; o += 4096
        tab_o = [o, o + 2048]; o += 4096
        tabm_o = o; o += 2048
        sc_o = [o + 512 * i for i in range(4)]; o += 2048
        pT_o = [o + 256 * i for i in range(4)]; o += 1024
        at_o = o; o += 1024
        abf_o = o; o += 512
        wattn_o = o; o += 1024
        junk2_o = o; o += 512
        tab_o += [o, o + 2048]; o += 4096
        assert o <= TOT
        aT = AB(aT_o, 4096).rearrange("p (c t) -> p c t", c=8)
        tabs = [A(tab_o[i], 2048) for i in range(4)]
        tabms = A(tabm_o, 2048)
        scs = [A(sc_o[i], 512) for i in range(4)]
        pTs = [AB(pT_o[i], 256) for i in range(4)]
        a_t = A(at_o, 1024)
        a_bf = AB(abf_o, 512)
        wattn = A(wattn_o, 1024)
        junk2 = AB(junk2_o, 512)
        P.dma('sp', lambda e: e.dma_start(out=tabms[0:16, :], in_=tabm[:, :]), 'k8', writes=['tabm'])
        P.dma('sp', lambda e: e.dma_start(out=wattn, in_=wbc_attn[:, :]), 'k9', writes=['wattn'])
        slot_list = []
        for j in range(8):
            for si, kc in enumerate(SLOTS[j]):
                slot_list.append((j, si, kc, SLOT_BASE[j] + si))

        def load_tab(n):
            gi = slot_list[n][3]
            s = n % 4
            P.dma('sp', lambda e, s=s, gi=gi: e.dma_start(out=tabs[s], in_=tab[gi, :, :]), 'tab%d' % s,
                  writes=[('tab', s)])

        wo_o = [R1 + 16384, aT_o + 4096]
        assert wo_o[0] + 4096 + 1024 <= R2
        wob = [AB(wo_o[i], 4096).rearrange("p (k c) -> p k c", k=16) for i in range(2)]
        junkO = AB(wo_o[0] + 4096, 1024)
        w_out_v = w_out.rearrange("(k p) c -> p k c", p=128)

        def load_wo(cb):
            s = cb % 2
            P.dma_group('pool', [lambda e, s=s, cb=cb, q4=q4: e.dma_start(
                out=wob[s][:, q4 * 4:(q4 + 1) * 4, :], in_=w_out_v[:, q4 * 4:(q4 + 1) * 4, cb * 512:(cb + 1) * 512]) for q4 in range(4)],
                ['wo%d_%d' % (s, q4) for q4 in range(4)], writes=[('wo', s)])

        if stage >= 4:
            load_wo(0)
        if stage >= 3:
            for n0 in range(4):
                load_tab(n0)
        n = 0
        scr = [0]
        pend = []
        for j in range(8 if stage >= 3 else 0):
            entries = [(kc, False) for kc in SLOTS[j]] + [('M', True)]
            for si, (kc, is_meta) in enumerate(entries):
                first = (si == 0)
                last = is_meta
                if is_meta:
                    kcol, KR, vch, tb_ap, tkey = 1280, 16, 10, tabms, 'tabm'
                else:
                    if kc == 'A':
                        kcol, vch = 1024, 8
                    elif kc == 'B':
                        kcol, vch = 1152, 9
                    else:
                        kcol, vch = kc * 128, kc
                    KR = 128
                    ts_ = n % 4
                    tb_ap, tkey = tabs[ts_], ('tab', ts_)
                for hg in range(4):
                    sbk = scr[0] % 4
                    u = scr[0] % 4
                    scr[0] += 1
                    p0 = 64 * (hg % 2)
                    for h4 in range(4):
                        ct_ = 4 * (hg // 2) + h4
                        P.op('pe', lambda e, sbk=sbk, h4=h4, ct_=ct_, p0=p0, kcol=kcol, KR=KR, j=j: e.matmul(
                            ps[:KR, sbk, h4 * 128:(h4 + 1) * 128], kT[p0:p0 + 64, ct_, kcol:kcol + KR],
                            qT[p0:p0 + 64, ct_, j * 128:(j + 1) * 128], start=True, stop=True),
                            reads=['qkT'], writes=[('ps', sbk)], mark=(h4 == 3))
                    P.op('dve', lambda e, sbk=sbk, u=u, KR=KR, tb_ap=tb_ap, hg=hg: e.tensor_tensor(
                        scs[u][:KR, :], ps[:KR, sbk, :], tb_ap[:KR, hg * 512:(hg + 1) * 512], ALU.add),
                        reads=[('ps', sbk), tkey], writes=[('sc', u)])
                    P.op('act', lambda e, u=u, KR=KR: e.activation(pTs[u][:KR, :], scs[u][:KR, :], AF.Exp),
                         reads=[('sc', u)], writes=[('pT', u)])

                    def emit_pv(u=u, KR=KR, vch=vch, hg=hg, first=first, last=last):
                        for h4 in range(4):
                            h = 8 * (hg // 2) + 2 * h4 + (hg % 2)
                            P.op('pe', lambda e, u=u, h4=h4, h=h, KR=KR, vch=vch, hg=hg, first=first, last=last: e.matmul(
                                ps[:, 4 + hg, h4 * 66:(h4 + 1) * 66], pTs[u][:KR, h4 * 128:(h4 + 1) * 128],
                                Vv[:KR, vch, h, :], start=(first and h4 == 0), stop=last, skip_group_check=True),
                                reads=[('pT', u), 'V'], writes=[('ps', 4 + hg)], mark=(h4 == 3))
                    pend.append(emit_pv)
                    while sum(1 for f in pend if f.__name__ == 'emit_pv') > 2:
                        pend.pop(0)()
                if not is_meta:
                    n += 1
                    if n + 3 < len(slot_list):
                        load_tab(n + 3)
            def norm(j=j):
                for hg in range(4):
                    ov = ps[:, 4 + hg, 0:264].rearrange("p (h d) -> p h d", h=4)
                    P.op('dve', lambda e, ov=ov, hg=hg: e.reciprocal(rec4[:, hg * 4:hg * 4 + 4].rearrange("p (h o) -> p h o", o=1), ov[:, :, 64:65]),
                         reads=[('ps', 4 + hg)], writes=[('rec', hg)])
                    abase = (8 * (hg // 2) + (hg % 2)) * 64
                    P.op('dve', lambda e, ov=ov, hg=hg, abase=abase: e.tensor_tensor(
                        a_t.rearrange("p (g x) -> p g x", x=128)[:, 4 * (hg // 2):4 * (hg // 2) + 4, (hg % 2) * 64:(hg % 2) * 64 + 64], ov[:, :, 0:64],
                        rec4[:, hg * 4:hg * 4 + 4].rearrange("p (h o) -> p h o", o=1).to_broadcast([128, 4, 64]), ALU.mult),
                        reads=[('ps', 4 + hg), ('rec', hg)], writes=['a_t'])
                P.op('act', lambda e, j=j: e.activation(junk2, a_t, AF.Square, accum_out=ssa[:, j:j + 1]),
                     reads=['a_t'], writes=['junk2', ('ssa', j)])
                P.op('dve', lambda e: e.tensor_tensor(a_bf, a_t, wattn, ALU.mult), reads=['a_t', 'wattn'], writes=['a_bf'])
                tbk = scr[0] % 4
                scr[0] += 1
                for c8 in range(8):
                    P.op('pe', lambda e, tbk=tbk, c8=c8: e.transpose(psb(tbk)[:, c8 * 128:(c8 + 1) * 128],
                                                                     a_bf[:, c8 * 128:(c8 + 1) * 128], identb),
                         reads=['a_bf', 'const'], writes=[('ps', tbk)], mark=(c8 == 7))
                P.op('act', lambda e, tbk=tbk, j=j: e.copy(aT[:, :, j * 128:(j + 1) * 128],
                                                            psb(tbk).rearrange("p (c t) -> p c t", c=8)),
                     reads=[('ps', tbk)], writes=['aT'])
            pend.append(norm)
        while pend:
            pend.pop(0)()
        P.op('act', lambda e: e.activation(ra, ssa, AF.Sqrt, bias=EPS, scale=1.0 / 1024),
             reads=[('ssa', j) for j in range(8)], writes=['ra'])
        P.op('dve', lambda e: e.reciprocal(ra, ra), reads=['ra'], writes=['ra'])
        P.barrier()

        h1 = A(R1, 16384).rearrange("p (j d) -> p j d", j=8)
        o = aT_o + 4096
        o += 8192
        xc_o = [o, o + 512]; o += 1024
        tmp_o = o; o += 512
        assert o <= TOT
        xcs = [A(xc_o[i], 512) for i in range(2)]
        tmpc = A(tmp_o, 512)

        RING_FIX = TOT - 8 * 1024
        assert o <= RING_FIX, o
        if stage >= 4:
            load_wo(1)
        moe_state = None
        if moe:
            moe_state = moe_ring(P, AB, RING_FIX, R1 + 16384, w_gate, w_up, w_down)
            for ci in range(8):
                moe_state['load'](ci)
        it = 0
        for cb in range(4 if stage >= 4 else 0):
            s = cb % 2
            for j in range(8):
                xs_ = it % 2
                P.dma('sp', lambda e, xs_=xs_, j=j, cb=cb: e.dma_start(
                    out=xcs[xs_], in_=xo[j * 128:(j + 1) * 128, cb * 512:(cb + 1) * 512]), 'xc%d' % xs_,
                    writes=[('xc', xs_)])
                ba, bc = (it % 4) * 2, (it % 4) * 2 + 1
                for k in range(8):
                    P.op('pe', lambda e, ba=ba, k=k, j=j, s=s: e.matmul(
                        ps[:, ba, :], aT[:, k, j * 128:(j + 1) * 128], wob[s][:, k, :], start=(k == 0), stop=(k == 7)),
                        reads=[('wo', s), 'aT'], writes=[('ps', ba)], mark=(k == 7))
                for k in range(8):
                    P.op('pe', lambda e, bc=bc, k=k, j=j, s=s: e.matmul(
                        ps[:, bc, :], cT[:, k, j * 128:(j + 1) * 128], wob[s][:, 8 + k, :], start=(k == 0), stop=(k == 7)),
                        reads=[('wo', s), 'cT'], writes=[('ps', bc)], mark=(k == 7))
                P.op('dve', lambda e, ba=ba, xs_=xs_, j=j: e.scalar_tensor_tensor(
                    tmpc, ps[:, ba, :], ra[:, j:j + 1], xcs[xs_], ALU.mult, ALU.add),
                    reads=[('ps', ba), ('xc', xs_)], writes=['tmpc'])
                P.op('dve', lambda e, bc=bc, j=j, cb=cb: e.scalar_tensor_tensor(
                    h1[:, j, cb * 512:(cb + 1) * 512], ps[:, bc, :], rc[:, j:j + 1], tmpc, ALU.mult, ALU.add),
                    reads=[('ps', bc), 'tmpc'], writes=[('h1', j)])
                if cb == 3 and moe:
                    P.op('act', lambda e, j=j: e.activation(junkO, h1[:, j, :], AF.Square, accum_out=ssf[:, j:j + 1]),
                         reads=[('h1', j)], writes=['junkO', ('ssf', j)])
                it += 1
            if cb + 2 < 4:
                load_wo(cb + 2)
        P.barrier()

        if moe:
            moe_phase(nc, P, A, AB, ps, psb, h1, R1 + 16384, TOT, cp, identf, identb, iotar, brt_s, ssf, rf, st,
                      wbc_ffn, wr, ccm, rsel, moe_state)

        for j in range(8):
            P.dma('sp', lambda e, j=j: e.dma_start(out=out[j * 128:(j + 1) * 128, :], in_=h1[:, j, :]), 'outs%d' % j,
                  reads=[('h1', j)])
        P.barrier()
        P.ops['sp'].append((None, P._waits('sp', P._deps('sp', (), ())), None, 0))

        sems = {}
        for name in P.sem_names():
            sems[name] = es.enter_context(nc.semaphore("s_" + name))
        block = es.enter_context(nc.Block())

        def run(eng, e):
            for fn, waits, inc, amt in P.ops[eng]:
                for (s_, v) in waits:
                    e.wait_ge(sems[s_], v)
                if fn is None:
                    continue
                ins = fn(e)
                if inc is not None:
                    ins.then_inc(sems[inc], amt)

        @block.tensor
        def _(e):
            run('pe', e)

        @block.scalar
        def _(e):
            run('act', e)

        @block.vector
        def _(e):
            run('dve', e)

        @block.gpsimd
        def _(e):
            run('pool', e)

        @block.sync
        def _(e):
            run('sp', e)
    return nc


NR = 15


def moe_ring(P, AB, ring_fix, base, w_gate, w_up, w_down):
    pers = 8192 + 2048 + 1024 + 1024 + 1024 + 512 + 512 + 1024 + 256 + 256 + 2048 + 512
    late_o = base + pers
    assert late_o + 7 * 1024 <= ring_fix, (late_o, ring_fix)
    offs = [ring_fix + i * 1024 for i in range(8)] + [late_o + i * 1024 for i in range(7)]
    ring = [AB(offs[i], 1024) for i in range(NR)]
    chunks = []
    for ex in range(NE):
        for k in range(16):
            chunks.append((ex, 'gu', k))
        for k in range(8):
            chunks.append((ex, 'd', k))

    def load(ci):
        ex, typ, k = chunks[ci]
        s = ci % NR
        if typ == 'gu':
            P.dma_group('pool', [
                lambda e, s=s, ex=ex, k=k: e.dma_start(out=ring[s][:, 0:1024], in_=w_gate[ex, k * 128:(k + 1) * 128, :]),
                lambda e, s=s, ex=ex, k=k: e.dma_start(out=ring[s][:, 1024:2048], in_=w_up[ex, k * 128:(k + 1) * 128, :])],
                ['rg%da' % s, 'rg%db' % s], writes=[('ring', s)])
        else:
            P.dma('pool', lambda e, s=s, ex=ex, k=k: e.dma_start(out=ring[s], in_=w_down[ex, k * 128:(k + 1) * 128, :]),
                  'rg%da' % s, writes=[('ring', s)])
    return {'ring': ring, 'chunks': chunks, 'load': load, 'late_o': late_o}


def moe_phase(nc, P, A, AB, ps, psb, h1, base, TOT, cp, identf, identb, iotar, brt_s, ssf, rf, st,
              wbc_ffn, wr, ccm, rsel, ms):
    BIG = 1000.0
    ring, chunks, load_chunk = ms['ring'], ms['chunks'], ms['load']
    o = base
    hn_o = o; o += 8192
    xe_o = [o, o + 1024]; o += 2048
    S_o = [o, o + 512]; o += 1024
    ST_o = [o, o + 512]; o += 1024
    sg_o = o; o += 1024
    H_o = o; o += 512
    HT_o = o; o += 512
    Y_o = o; o += 1024
    Wt_o = o; o += 256
    posm_o = o; o += 256
    rs_o = o; o += 2048
    ptm_o = o; o += 512
    assert o == ms['late_o'], (o, ms['late_o'])
    junk3_o = o; o += 1024
    wr_o = o; o += 576
    lg_o = o; o += 288
    sm_o = o; o += 1600
    cc_o = o; o += 960
    ptf_o = o; o += 1024
    mt_o = o; o += 1024
    assert o <= ms['late_o'] + 7 * 1024, o
    hn32 = A(xe_o[0], 2048)
    hnT = A(xe_o[0] + 2048, 2048).rearrange("p (k t) -> p k t", k=16)
    wffn = A(xe_o[0] + 4096, 2048)
    assert xe_o[0] + 6144 <= Wt_o
    junk3 = AB(junk3_o, 1024)

    hn = AB(hn_o, 8192).rearrange("p (j d) -> p j d", j=8)
    xe = [AB(xe_o[i], 1024).rearrange("p (k s) -> p k s", k=16) for i in range(2)]
    Sb = [AB(S_o[i], 512).rearrange("p (j s) -> p j s", j=8) for i in range(2)]
    STb = [AB(ST_o[i], 512) for i in range(2)]
    sg = A(sg_o, 1024)
    Hb = AB(H_o, 512)
    HTb = AB(HT_o, 512).rearrange("p (k s) -> p k s", k=8)
    Yb = AB(Y_o, 1024)
    wrs = A(wr_o, 576).rearrange("p (k n) -> p k n", k=16)
    lg = A(lg_o, 288)
    L3 = lg.rearrange("p (j n) -> p j n", j=8)
    sm = A(sm_o, 1600)
    posm = A(posm_o, 256)
    Wt = A(Wt_o, 256)
    ccb = AB(cc_o, 960)
    rsb_ = AB(rs_o, 2048)
    ptm = AB(ptm_o, 512)
    ptf = A(ptf_o, 1024)
    mts = A(mt_o, 1024)

    P.dma('sp', lambda e: e.dma_start(out=wffn, in_=wbc_ffn[:, :]), 'k10', writes=['wffn'])
    P.dma('sp', lambda e: e.dma_start(out=A(wr_o, 576), in_=wr[:, :]), 'k11', writes=['wrs'])
    P.dma('pool', lambda e: e.dma_start(out=ccb, in_=ccm[:, :]), 'k12', writes=['ccb'])
    P.dma('pool', lambda e: e.dma_start(out=rsb_[0:32, :], in_=rsel[:, :]), 'k13', writes=['rsel'])

    P.op('act', lambda e: e.activation(rf, ssf, AF.Sqrt, bias=EPS, scale=1.0 / 2048),
         reads=[('ssf', j) for j in range(8)], writes=['rf'])
    P.op('dve', lambda e: e.reciprocal(rf, rf), reads=['rf'], writes=['rf'])
    for j in range(8):
        P.op('dve', lambda e, j=j: e.scalar_tensor_tensor(hn32, h1[:, j, :], rf[:, j:j + 1], wffn, ALU.mult, ALU.mult),
             reads=[('h1', j), 'rf', 'wffn'], writes=['hn32'])
        P.op('act', lambda e, j=j: e.copy(hn[:, j, :], hn32), reads=['hn32'], writes=['hn'])
        for g in range(4):
            for kk in range(4):
                k = g * 4 + kk
                P.op('pe', lambda e, g=g, kk=kk, k=k: e.transpose(ps[:, g, kk * 128:(kk + 1) * 128],
                                                                   hn32[:, k * 128:(k + 1) * 128], identf),
                     reads=['hn32', 'const'], writes=[('ps', g)], mark=(kk == 3))
            if g % 2 == 0:
                P.op('act', lambda e, g=g: e.copy(hnT[:, g * 4:g * 4 + 4, :], ps[:, g, :].rearrange("p (k t) -> p k t", k=4)),
                     reads=[('ps', g)], writes=['hnT'])
            else:
                P.op('dve', lambda e, g=g: e.tensor_copy(hnT[:, g * 4:g * 4 + 4, :], ps[:, g, :].rearrange("p (k t) -> p k t", k=4)),
                     reads=[('ps', g)], writes=['hnT'])
        for k in range(16):
            P.op('pe', lambda e, k=k, j=j: e.matmul(ps[:, 4, j * 36:(j + 1) * 36], hnT[:, k, :], wrs[:, k, :],
                                                    start=(k == 0), stop=(k == 15)),
                 reads=['hnT', 'wrs'], writes=[('ps', 4)], mark=(k == 15))
    P.op('dve', lambda e: e.tensor_tensor(lg, ps[:, 4, 0:288], brt_s, ALU.add), reads=[('ps', 4), 'const'], writes=['lg'])

    gmax = sm[:, 0:8]
    gm = sm[:, 8:40].rearrange("p (j g) -> p j g", j=8)
    gd = sm[:, 40:72].rearrange("p (j g) -> p j g", j=8)
    gs = sm[:, 72:80]
    gw = sm[:, 80:88]
    lem = sm[:, 96:352]
    lem4 = lem.rearrange("p (j g x) -> p j g x", j=8, g=4)
    lem3 = lem.rearrange("p (j n) -> p j n", j=8)
    mx = sm[:, 352:416].rearrange("p (j x) -> p j x", j=8)
    sel = sm[:, 416:672]
    sel3 = sel.rearrange("p (j n) -> p j n", j=8)
    pe_ = sm[:, 672:928]
    pe3 = pe_.rearrange("p (j n) -> p j n", j=8)
    e2 = sm[:, 928:936]
    scl = sm[:, 936:944]
    Mbf = A(sm_o + 944, 128).bitcast(BF16).rearrange("p (j n) -> p j n", j=8)
    pos_t = sm[:, 1072:1328]
    iop = cp[:, C_IOP:C_IOP + 1]

    def b3(ap2, n):
        return ap2.rearrange("p (j o) -> p j o", o=1).to_broadcast([128, 8, n])

    def rt(eng, fn):
        P.op(eng, fn, reads=['rt', 'lg'], writes=['rt'])

    rt('dve', lambda e: e.tensor_reduce(gmax, L3[:, :, 0:4], AX.X, ALU.max))
    rt('dve', lambda e: e.tensor_tensor(gm, L3[:, :, 0:4], b3(gmax, 4), ALU.is_ge))
    rt('dve', lambda e: e.tensor_tensor(gd, L3[:, :, 0:4], b3(gmax, 4), ALU.subtract))
    rt('act', lambda e: e.activation(gd, gd, AF.Exp))
    rt('dve', lambda e: e.tensor_reduce(gs, gd, AX.X, ALU.add))
    rt('dve', lambda e: e.reciprocal(gw, gs))
    rt('dve', lambda e: e.tensor_scalar(gm, gm, BIG, -BIG, ALU.mult, ALU.add))
    rt('dve', lambda e: e.tensor_tensor(
        lem4, L3[:, :, 4:36].rearrange("p j (g x) -> p j g x", g=4),
        gm.rearrange("p j (g o) -> p j g o", o=1).to_broadcast([128, 8, 4, 8]), ALU.add))
    for j in range(8):
        rt('dve', lambda e, j=j: e.max(out=mx[:, j, :], in_=lem3[:, j, :]))
    rt('dve', lambda e: e.tensor_tensor(sel3, lem3, mx[:, :, 1:2].to_broadcast([128, 8, 32]), ALU.is_ge))
    rt('dve', lambda e: e.tensor_tensor(pe3, lem3, mx[:, :, 0:1].to_broadcast([128, 8, 32]), ALU.subtract))
    rt('act', lambda e: e.activation(pe_, pe_, AF.Exp))
    rt('dve', lambda e: e.tensor_tensor(pe_, pe_, sel, ALU.mult))
    rt('dve', lambda e: e.tensor_tensor(e2.rearrange("p (j o) -> p j o", o=1), mx[:, :, 1:2], mx[:, :, 0:1], ALU.subtract))
    rt('act', lambda e: e.activation(e2, e2, AF.Exp))
    rt('dve', lambda e: e.tensor_scalar(e2, e2, 1.0, None, ALU.add))
    rt('dve', lambda e: e.reciprocal(e2, e2))
    rt('dve', lambda e: e.tensor_tensor(scl, gw, e2, ALU.mult))
    rt('dve', lambda e: e.tensor_tensor(Wt.rearrange("p (j n) -> p j n", j=8), pe3, b3(scl, 32), ALU.mult))
    rt('dve', lambda e: e.tensor_copy(Mbf, sel3))

    for j in range(8):
        for n2 in range(2):
            P.op('pe', lambda e, j=j, n2=n2: e.matmul(
                ps[0:32, n2, :], Mbf[:, j, :], ccb[:, (7 - j) * 128 + n2 * 512:(7 - j) * 128 + n2 * 512 + 512],
                start=(j == 0), stop=(j == 7)),
                reads=['rt', 'ccb'], writes=[('ps', n2)], mark=(j == 7))
    for j in range(8):
        b = 2 + j // 4
        P.op('pe', lambda e, j=j, b=b: e.transpose(ps[0:32, b, (j % 4) * 128:(j % 4 + 1) * 128], sel3[:, j, :], identf),
             reads=['rt', 'const'], writes=[('ps', b)], mark=(j % 4 == 3))
    for n2 in range(2):
        P.op('act', lambda e, n2=n2: e.copy(mts[0:32, n2 * 512:(n2 + 1) * 512], ps[0:32, 2 + n2, :]),
             reads=[('ps', 2 + n2)], writes=['mts'])
        P.op('dve', lambda e, n2=n2: e.scalar_tensor_tensor(
            ptf[0:32, n2 * 512:(n2 + 1) * 512], ps[0:32, n2, :], 1.0, mts[0:32, n2 * 512:(n2 + 1) * 512], ALU.add, ALU.mult),
            reads=[('ps', n2), 'mts'], writes=['ptf'])
    P.op('dve', lambda e: e.tensor_scalar(ptf[0:32, :], ptf[0:32, :], -1.0, None, ALU.add), reads=['ptf'], writes=['ptf'])
    P.op('act', lambda e: e.copy(ptm[0:32, :], ptf[0:32, :]), reads=['ptf'], writes=['ptm'])
    for j in range(8):
        P.op('pe', lambda e, j=j: e.transpose(ps[:, 4, j * 32:(j + 1) * 32], ptf[0:32, j * 128:(j + 1) * 128], identf[0:32, 0:32]),
             reads=['ptf', 'const'], writes=[('ps', 4)], mark=(j == 7))
    P.op('dve', lambda e: e.tensor_copy(posm, ps[:, 4, 0:256]), reads=[('ps', 4)], writes=['posm'])
    posm3 = posm.rearrange("p (j n) -> p j n", j=8)
    Wt3 = Wt.rearrange("p (j n) -> p j n", j=8)
    P.barrier()

    for ci in range(8, NR):
        load_chunk(ci)
    ci = 0
    rot = 0
    def build_S(ex):
        sl = ex % 2
        for j in range(8):
            P.op('dve', lambda e, j=j, ex=ex, sl=sl: e.tensor_scalar(Sb[sl][:, j, :], iotar, posm3[:, j, ex:ex + 1], None, ALU.is_equal),
                 reads=['posm', 'const'], writes=[('S', sl)])
        for n2 in range(2):
            b = 6 + n2
            P.op('pe', lambda e, n2=n2, b=b, ex=ex: e.matmul(ps[:, b, :], rsb_[0:32, ex * 128:(ex + 1) * 128],
                                                             ptm[0:32, n2 * 512:(n2 + 1) * 512], start=True, stop=True),
                 reads=['ptm', 'rsel'], writes=[('ps', b)])
            P.op('dve', lambda e, n2=n2, b=b, sl=sl: e.tensor_scalar(STb[sl][:, n2 * 512:(n2 + 1) * 512], ps[:, b, :], iop, None, ALU.is_equal),
                 reads=[('ps', b), 'const'], writes=[('ST', sl)])

    build_S(0)
    for ex in range(NE):
        sl = ex % 2
        for g in range(4):
            b = g % 2
            for kk in range(4):
                dk = g * 4 + kk
                for j in range(8):
                    P.op('pe', lambda e, b=b, kk=kk, dk=dk, j=j, sl=sl: e.matmul(
                        ps[:, b, kk * 128:(kk + 1) * 128], hn[:, j, dk * 128:(dk + 1) * 128], Sb[sl][:, j, :],
                        start=(j == 0), stop=(j == 7)),
                        reads=['hn', ('S', sl)], writes=[('ps', b)], mark=(kk == 3 and j == 7))
            P.op('act', lambda e, b=b, g=g, sl=sl: e.copy(xe[sl][:, g * 4:g * 4 + 4, :], ps[:, b, :].rearrange("p (k s) -> p k s", k=4)),
                 reads=[('ps', b)], writes=[('xe', sl)])
        for k in range(16):
            s = ci % NR
            for n4 in range(4):
                P.op('pe', lambda e, k=k, n4=n4, s=s, sl=sl: e.matmul(
                    ps[:, 2 + n4, :], xe[sl][:, k, :], ring[s][:, n4 * 512:(n4 + 1) * 512], start=(k == 0), stop=(k == 15)),
                    reads=[('xe', sl), ('ring', s)], writes=[('ps', 2 + n4)], mark=(n4 == 3))
            if ci + NR < len(chunks):
                load_chunk(ci + NR)
            ci += 1
        for n2 in range(2):
            P.op('act', lambda e, n2=n2: e.activation(sg[:, n2 * 512:(n2 + 1) * 512], ps[:, 2 + n2, :], AF.Silu),
                 reads=[('ps', 2 + n2)], writes=['sg'])
            P.op('dve', lambda e, n2=n2: e.tensor_tensor(Hb[:, n2 * 512:(n2 + 1) * 512], sg[:, n2 * 512:(n2 + 1) * 512], ps[:, 4 + n2, :], ALU.mult),
                 reads=[('ps', 4 + n2), 'sg'], writes=['H'])
        for k in range(8):
            P.op('pe', lambda e, k=k: e.transpose(psb(6)[:, k * 128:(k + 1) * 128], Hb[:, k * 128:(k + 1) * 128], identb),
                 reads=['H', 'const'], writes=[('ps', 6)], mark=(k == 7))
        P.op('act', lambda e: e.copy(HTb, psb(6).rearrange("p (k s) -> p k s", k=8)), reads=[('ps', 6)], writes=['HT'])
        for k in range(8):
            s = ci % NR
            for n4 in range(4):
                P.op('pe', lambda e, k=k, n4=n4, s=s: e.matmul(
                    ps[:, 2 + n4, :], HTb[:, k, :], ring[s][:, n4 * 512:(n4 + 1) * 512], start=(k == 0), stop=(k == 7)),
                    reads=['HT', ('ring', s)], writes=[('ps', 2 + n4)], mark=(n4 == 3))
            if ci + NR < len(chunks):
                load_chunk(ci + NR)
            ci += 1
        if ex + 1 < NE:
            build_S(ex + 1)
        for n4 in range(4):
            if n4 % 2 == 0:
                P.op('act', lambda e, n4=n4: e.copy(Yb[:, n4 * 512:(n4 + 1) * 512], ps[:, 2 + n4, :]),
                     reads=[('ps', 2 + n4)], writes=['Y'])
            else:
                P.op('dve', lambda e, n4=n4: e.tensor_copy(Yb[:, n4 * 512:(n4 + 1) * 512], ps[:, 2 + n4, :]),
                     reads=[('ps', 2 + n4)], writes=['Y'])
        sbanks = [0, 1, 6, 7]
        for j in range(8):
            for n4 in range(4):
                b = sbanks[rot % 4]
                rot += 1
                P.op('pe', lambda e, b=b, j=j, n4=n4, sl=sl: e.matmul(
                    ps[:, b, :], STb[sl][:, j * 128:(j + 1) * 128], Yb[:, n4 * 512:(n4 + 1) * 512], start=True, stop=True),
                    reads=[('ST', sl), 'Y'], writes=[('ps', b)])
                P.op('dve', lambda e, b=b, j=j, n4=n4, ex=ex: e.scalar_tensor_tensor(
                    h1[:, j, n4 * 512:(n4 + 1) * 512], ps[:, b, :], Wt3[:, j, ex:ex + 1], h1[:, j, n4 * 512:(n4 + 1) * 512],
                    ALU.mult, ALU.add),
                    reads=[('ps', b), 'rt'], writes=[('h1', j)])
    P.barrier()


def _bias_tables(rel_bias, hf):
    nh = rel_bias.shape[0]
    c = np.arange(64)
    col_start = np.clip(c - 8, 0, 48)
    cmask = (c[None, :] >= col_start[:, None]) & (c[None, :] < col_start[:, None] + 16)
    dc = np.clip(c[None, :] - c[:, None], -15, 15) + 15
    tabs = np.full((NSLOT, 2, 64, nh, 2, 64), NEG, np.float32)
    glob = {'A': 8 if hf == 0 else 6, 'B': 9 if hf == 0 else 7}
    for j in range(8):
        gq = 8 * hf + j
        for si, kc in enumerate(SLOTS[j]):
            gc = glob[kc] if isinstance(kc, str) else 8 * hf + kc
            t = tabs[SLOT_BASE[j] + si]
            for rqp in range(2):
                rq = 2 * gq + rqp
                start = min(max(rq - 4, 0), 24)
                for rkp in range(2):
                    rk = 2 * gc + rkp
                    if not (start <= rk < start + 8):
                        continue
                    dr = rk - rq + 7
                    blk = rel_bias[:, dr, :][:, dc]
                    blk = np.where(cmask[None], blk, np.float32(NEG))
                    t[rkp, :, :, rqp, :] = blk.transpose(2, 0, 1)
    tabs = tabs[:, :, :, HORDER]
    return tabs.reshape(NSLOT, 128, nh * 128)


def _prep(x, meta_tokens, mix_norm_w, w_in, q_norm_w, k_norm_w, rel_bias, meta_bias, conv_w,
          attn_out_norm_w, conv_out_norm_w, w_out, ffn_norm_w, w_router_group, b_router_group,
          w_router_expert, b_router_expert, w_gate, w_up, w_down, moe=True):
    f = lambda a: np.ascontiguousarray(np.asarray(a), dtype=np.float32)
    x = f(x); meta = f(meta_tokens)
    p = np.arange(128)
    cpack = np.zeros((128, NCP), np.float32)
    cpack[:, C_WQ] = f(q_norm_w)[0][p % 64]
    cpack[:, C_WK] = f(k_norm_w)[0][p % 64]
    cw = f(conv_w)[0]
    for ct in range(8):
        for tap in range(3):
            cpack[:, C_CW + ct * 3 + tap] = cw[tap, ct * 128:(ct + 1) * 128]
        cpack[:, C_WC + ct] = f(conv_out_norm_w)[0][ct * 128:(ct + 1) * 128]
    cpack[:, C_IOP] = p
    cpack[:, C_ONE] = 1.0
    rep = lambda v: np.ascontiguousarray(np.broadcast_to(f(v).reshape(1, -1), (128, f(v).size)))
    bcat = np.concatenate([f(b_router_group)[0], f(b_router_expert)[0]])
    wcat = np.concatenate([f(w_router_group)[0], f(w_router_expert)[0]], axis=1)
    cmat = np.zeros((128, 384), np.float32)
    cmat[:, 0:128] = np.eye(128)
    cmat[:, 128:256] = np.kron(np.eye(2), np.ones((64, 64)))
    cmat[:, 256:384] = np.arange(128)[None, :]
    ccm = np.zeros((128, 1920), np.float32)
    ccm[:, 896:1024] = (p[:, None] < p[None, :])
    ccm[:, 1024:] = 1.0
    rsel = np.zeros((32, NE, 128), np.float32)
    rsel[np.arange(32), np.arange(32), :] = 1.0
    mb = f(meta_bias)[0]
    tabm = np.ascontiguousarray(np.broadcast_to(mb.T[:, HORDER, None], (16, 16, 128))).reshape(16, 2048)
    shared = {
        "w_in": f(w_in)[0], "w_out": f(w_out)[0], "tabm": tabm, "cpack": cpack,
        "wbc_mix": rep(mix_norm_w), "wbc_attn": rep(attn_out_norm_w), "wbc_ffn": rep(ffn_norm_w),
        "brt": rep(np.tile(bcat, 8)),
        "wr": np.ascontiguousarray(wcat.reshape(16, 128, 36).transpose(1, 0, 2)).reshape(128, 576),
        "cmat": cmat, "ccm": ccm, "rsel": rsel.reshape(32, NE * 128),
    }
    if moe:
        shared.update({"w_gate": f(w_gate)[0], "w_up": f(w_up)[0], "w_down": f(w_down)[0]})
    tabs = [_bias_tables(f(rel_bias)[0], hf) for hf in range(2)]
    in_maps = []
    for c in range(8):
        b, hf = c // 2, c % 2
        xg = np.zeros((288, 2048), np.float32)
        if hf == 0:
            xg[0:256] = x[b, 1024:1280]
            xg[272] = meta[15]
            xg[273] = x[b, 1024]
        else:
            xg[0:256] = x[b, 768:1024]
            xg[272] = x[b, 1023]
        xg[256:272] = meta
        m = dict(shared)
        m["xo"] = np.ascontiguousarray(x[b, hf * 1024:(hf + 1) * 1024])
        m["xg"] = xg
        m["tab"] = tabs[hf]
        in_maps.append(m)
    return in_maps


_NC_CACHE = {}


def kernel(moe=True, stage=9, **inputs):
    in_maps = _prep(moe=moe, **inputs)
    if (moe, stage) not in _NC_CACHE:
        _NC_CACHE[(moe, stage)] = build(moe=moe, stage=stage)
    res = run_bass_kernel_spmd(_NC_CACHE[(moe, stage)], in_maps, core_ids=list(range(8)))
    out = np.zeros((4, 2048, 2048), np.float32)
    for c in range(8):
        b, hf = c // 2, c % 2
        out[b, hf * 1024:(hf + 1) * 1024] = res.results[c]["out"]
    return out
```
